# Optimizing a Trainium2 kernel written in Bass

```python
import jax
import jax.numpy as jnp
from jax import lax
import numpy as np

D_MODEL = 1024
BATCH = 8
SEQ = 4096
DEPTH = 2

N_MIXERS = 2
N_A_LAYERS = (DEPTH + 1) // 2
N_B_LAYERS = DEPTH // 2
RMS_EPS = 1e-6
DILATION_PATTERNS = ((128, 1), (512, 4), (2048, 16))
N_GROUPS_A = 3
HEADS_A = 16
HEAD_DIM_A = D_MODEL // HEADS_A
WIDTH_A = HEADS_A * HEAD_DIM_A
BAND_BLOCK = 128
ROPE_THETA = 10000.0
MAX_POS_OFFSET = 1024
NEG_INF = -1e30
HEADS_B = 4
KEY_DIM_B = D_MODEL // 2 // HEADS_B
VAL_DIM_B = D_MODEL // HEADS_B
HK_B = HEADS_B * KEY_DIM_B
HV_B = HEADS_B * VAL_DIM_B
GATE_RANK = 16
GATE_TAU = 16.0
GLA_CHUNK = 64
B_IN_WIDTH = 2 * HK_B + 2 * HV_B + GATE_RANK
N_EXPERTS = 32
TOP_K = 4
D_FF = D_MODEL
SWIGLU_LIMIT = 7.0
SWIGLU_ALPHA = 1.702
EXPERT_BLOCK = 128

kernel_name = 'hybrid_dilated_gla_moe_adaln'


def rmsnorm(t, gain):
    tf = t.astype(jnp.float32)
    y = tf * lax.rsqrt(jnp.mean(tf * tf, axis=-1, keepdims=True) + RMS_EPS)
    return (y * gain.astype(jnp.float32)).astype(t.dtype)


def rope(t, positions):
    half = t.shape[-1] // 2
    inv_freq = ROPE_THETA ** (-jnp.arange(half, dtype=jnp.float32) / half)
    ang = positions.astype(jnp.float32)[..., None] * inv_freq
    cos = jnp.cos(ang)[:, :, None, :]
    sin = jnp.sin(ang)[:, :, None, :]
    tf = t.astype(jnp.float32)
    t1, t2 = tf[..., :half], tf[..., half:]
    return jnp.concatenate([t1 * cos - t2 * sin, t2 * cos + t1 * sin], axis=-1).astype(t.dtype)


def dilated_window_attention(q, k, v, steps, dil):
    B, S, H, E = q.shape
    L = S // dil
    nb = -(-L // BAND_BLOCK)
    Lp = nb * BAND_BLOCK

    def residue_blocks(a):
        a = a.reshape(B, L, dil, H, E).transpose(0, 2, 1, 3, 4)
        a = jnp.pad(a, ((0, 0), (0, 0), (0, Lp - L), (0, 0), (0, 0)))
        return a.reshape(B, dil, nb, BAND_BLOCK, H, E)

    def with_prev(a):
        prev = jnp.pad(a[:, :, :-1], ((0, 0), (0, 0), (1, 0), (0, 0), (0, 0), (0, 0)))
        return jnp.concatenate([prev, a], axis=3)

    qb = residue_blocks(q)
    kk = with_prev(residue_blocks(k))
    vv = with_prev(residue_blocks(v))
    s = jnp.einsum('brnqhe,brnkhe->brnhqk', qb, kk,
                   preferred_element_type=jnp.float32) * (E ** -0.5)
    qi = jnp.arange(BAND_BLOCK)[:, None]
    kj = jnp.arange(2 * BAND_BLOCK)[None, :]
    delta = qi + BAND_BLOCK - kj
    blk = jnp.arange(nb)[:, None, None]
    valid = (delta >= 0) & (delta <= steps) & ((blk > 0) | (kj >= BAND_BLOCK))
    s = jnp.where(valid[:, None], s, NEG_INF)
    m = jnp.max(s, axis=-1, keepdims=True)
    p = jnp.exp(s - m)
    den = jnp.sum(p, axis=-1, keepdims=True)
    o = jnp.einsum('brnhqk,brnkhe->brnhqe', p, vv.astype(jnp.float32)) / den
    lse = (m + jnp.log(den))[..., 0]
    o = o.transpose(0, 1, 2, 4, 3, 5).reshape(B, dil, Lp, H, E)[:, :, :L]
    o = o.transpose(0, 2, 1, 3, 4).reshape(B, S, H, E)
    lse = lse.transpose(0, 1, 2, 4, 3).reshape(B, dil, Lp, H)[:, :, :L]
    lse = lse.transpose(0, 2, 1, 3).reshape(B, S, H)
    return o, lse


def dilated_mixer(h, positions, w_in, q_gain, k_gain, w_out):
    B, S, _ = h.shape
    qkv = (h @ w_in).reshape(B, S, N_GROUPS_A, 3, HEADS_A, HEAD_DIM_A)
    outs, lses = [], []
    for g, (window, dil) in enumerate(DILATION_PATTERNS):
        q = rope(rmsnorm(qkv[:, :, g, 0], q_gain[g]), positions)
        k = rope(rmsnorm(qkv[:, :, g, 1], k_gain[g]), positions)
        o, lse = dilated_window_attention(q, k, qkv[:, :, g, 2], window // dil, dil)
        outs.append(o)
        lses.append(lse)
    w = jax.nn.softmax(jnp.stack(lses, axis=0), axis=0)
    o = jnp.sum(w[..., None] * jnp.stack(outs, axis=0), axis=0)
    return o.astype(h.dtype).reshape(B, S, WIDTH_A) @ w_out


def gla_mixer(h, w_in, w_gate_up, gate_bias, out_gain, w_out):
    B, S, _ = h.shape
    nc = S // GLA_CHUNK
    proj = h @ w_in
    q, k, v, r, a = jnp.split(proj, [HK_B, 2 * HK_B, 2 * HK_B + HV_B, 2 * HK_B + 2 * HV_B], axis=-1)
    log_alpha = jax.nn.log_sigmoid((a @ w_gate_up + gate_bias).astype(jnp.float32)) / GATE_TAU

    def chunks(t, dim):
        return t.astype(jnp.float32).reshape(B, nc, GLA_CHUNK, HEADS_B, dim).transpose(0, 3, 1, 2, 4)

    q = chunks(q, KEY_DIM_B) * (KEY_DIM_B ** -0.5)
    k = chunks(k, KEY_DIM_B)
    v = chunks(v, VAL_DIM_B)
    b = jnp.cumsum(chunks(log_alpha, KEY_DIM_B), axis=3)
    b_last = b[:, :, :, -1:]
    q_dec = q * jnp.exp(b)
    att = jnp.einsum('bhncd,bhnjd->bhncj', q_dec, k * jnp.exp(-b))
    causal = jnp.tril(jnp.ones((GLA_CHUNK, GLA_CHUNK), dtype=bool))
    att = jnp.where(causal, att, 0.0)
    o_intra = jnp.einsum('bhncj,bhnjv->bhncv', att, v)
    k_dec = k * jnp.exp(b_last - b)
    chunk_decay = jnp.exp(b_last[:, :, :, 0])

    def step(state, inp):
        qd, kd, vc, dec = inp
        o = jnp.einsum('bhcd,bhdv->bhcv', qd, state)
        state = dec[..., None] * state + jnp.einsum('bhcd,bhcv->bhdv', kd, vc)
        return state, o

    xs = (jnp.moveaxis(q_dec, 2, 0), jnp.moveaxis(k_dec, 2, 0),
          jnp.moveaxis(v, 2, 0), jnp.moveaxis(chunk_decay, 2, 0))
    state0 = jnp.zeros((B, HEADS_B, KEY_DIM_B, VAL_DIM_B), jnp.float32)
    _, o_inter = lax.scan(step, state0, xs)
    o = o_intra + jnp.moveaxis(o_inter, 0, 2)
    o = o.transpose(0, 2, 3, 1, 4).reshape(B, S, HEADS_B, VAL_DIM_B)
    o = o * lax.rsqrt(jnp.mean(o * o, axis=-1, keepdims=True) + RMS_EPS) * out_gain.astype(jnp.float32)
    o = o.reshape(B, S, HV_B) * jax.nn.silu(r.astype(jnp.float32))
    return o.astype(h.dtype) @ w_out


def moe_ffn(h, router_w, router_b, w_gu, b_gu, w_down, b_down):
    B, S, D = h.shape
    n_tok = B * S
    t = h.reshape(n_tok, D)
    logits = (t @ router_w + router_b).astype(jnp.float32)
    top_v, top_i = lax.top_k(logits, TOP_K)
    gates = jax.nn.softmax(top_v, axis=-1)
    n_assign = n_tok * TOP_K
    flat_e = top_i.reshape(-1).astype(jnp.int32)
    flat_tok = jnp.repeat(jnp.arange(n_tok, dtype=jnp.int32), TOP_K)
    flat_w = gates.reshape(-1)
    order = jnp.argsort(flat_e)
    sorted_e = flat_e[order]
    counts = jnp.bincount(flat_e, length=N_EXPERTS)
    padded = (counts + EXPERT_BLOCK - 1) // EXPERT_BLOCK * EXPERT_BLOCK
    seg_end = jnp.cumsum(padded)
    rank = jnp.arange(n_assign, dtype=jnp.int32) - (jnp.cumsum(counts) - counts)[sorted_e]
    dest = (seg_end - padded)[sorted_e] + rank
    cap = n_assign + N_EXPERTS * EXPERT_BLOCK
    n_blocks = cap // EXPERT_BLOCK
    tok_buf = jnp.full((cap,), n_tok, jnp.int32).at[dest].set(flat_tok[order])
    w_buf = jnp.zeros((cap,), jnp.float32).at[dest].set(flat_w[order])
    block_e = jnp.minimum(
        jnp.searchsorted(seg_end, jnp.arange(n_blocks, dtype=jnp.int32) * EXPERT_BLOCK, side='right'),
        N_EXPERTS - 1)
    t_pad = jnp.concatenate([t, jnp.zeros((1, D), t.dtype)], axis=0)

    def expert_block(args):
        idx, e = args
        gu = (t_pad[idx] @ w_gu[e] + b_gu[e]).astype(jnp.float32)
        gate = jnp.minimum(gu[:, :D_FF], SWIGLU_LIMIT)
        up = jnp.clip(gu[:, D_FF:], -SWIGLU_LIMIT, SWIGLU_LIMIT)
        act = (up + 1.0) * (gate * jax.nn.sigmoid(SWIGLU_ALPHA * gate))
        return (act.astype(h.dtype) @ w_down[e] + b_down[e]).astype(jnp.float32)

    outs = lax.map(expert_block, (tok_buf.reshape(n_blocks, EXPERT_BLOCK), block_e))
    outs = outs.reshape(cap, D) * w_buf[:, None]
    y = jnp.zeros((n_tok + 1, D), jnp.float32).at[tok_buf].add(outs)[:n_tok]
    return y.astype(h.dtype).reshape(B, S, D)


def setup_inputs(seed: int = 0) -> dict:
    key = jax.random.key(seed)
    ks = jax.random.split(key, 22)
    D, E, F = D_MODEL, N_EXPERTS, D_FF

    def nrm(k, shape, scale):
        return jax.random.normal(k, shape, jnp.float32) * scale

    offsets = jax.random.randint(ks[2], (BATCH, 1), 0, MAX_POS_OFFSET, dtype=jnp.int32)
    positions = (offsets + jnp.arange(SEQ, dtype=jnp.int32)[None, :]).astype(jnp.int32)
    return {
        'x': nrm(ks[0], (BATCH, SEQ, D), 1.0),
        'c': nrm(ks[1], (BATCH, D), 1.0),
        'positions': positions,
        'ada_w': nrm(ks[3], (DEPTH, D, 6 * D), 0.5 * D ** -0.5),
        'ada_b': nrm(ks[4], (DEPTH, 6 * D), 0.02),
        'norm1_g': 1.0 + nrm(ks[5], (DEPTH, D), 0.02),
        'norm2_g': 1.0 + nrm(ks[6], (DEPTH, D), 0.02),
        'a_w_in': nrm(ks[7], (N_A_LAYERS, D, N_GROUPS_A * 3 * WIDTH_A), D ** -0.5),
        'a_q_gain': 1.0 + nrm(ks[8], (N_A_LAYERS, N_GROUPS_A, HEAD_DIM_A), 0.02),
        'a_k_gain': 1.0 + nrm(ks[9], (N_A_LAYERS, N_GROUPS_A, HEAD_DIM_A), 0.02),
        'a_w_out': nrm(ks[10], (N_A_LAYERS, WIDTH_A, D), WIDTH_A ** -0.5),
        'b_w_in': nrm(ks[11], (N_B_LAYERS, D, B_IN_WIDTH), D ** -0.5),
        'b_w_gate_up': nrm(ks[12], (N_B_LAYERS, GATE_RANK, HK_B), GATE_RANK ** -0.5),
        'b_gate_bias': nrm(ks[13], (N_B_LAYERS, HK_B), 0.1),
        'b_out_gain': 1.0 + nrm(ks[14], (N_B_LAYERS, VAL_DIM_B), 0.02),
        'b_w_out': nrm(ks[15], (N_B_LAYERS, HV_B, D), HV_B ** -0.5),
        'router_w': nrm(ks[16], (DEPTH, D, E), D ** -0.5),
        'router_b': nrm(ks[17], (DEPTH, E), 0.01),
        'moe_w_gu': nrm(ks[18], (DEPTH, E, D, 2 * F), D ** -0.5),
        'moe_b_gu': nrm(ks[19], (DEPTH, E, 2 * F), 0.02),
        'moe_w_down': nrm(ks[20], (DEPTH, E, F, D), F ** -0.5),
        'moe_b_down': nrm(ks[21], (DEPTH, E, D), 0.02),
    }


def reference(x, c, positions, ada_w, ada_b, norm1_g, norm2_g, a_w_in, a_q_gain, a_k_gain,
              a_w_out, b_w_in, b_w_gate_up, b_gate_bias, b_out_gain, b_w_out, router_w,
              router_b, moe_w_gu, moe_b_gu, moe_w_down, moe_b_down):
    cond = jax.nn.silu(c)
    for layer in range(DEPTH):
        mod = cond @ ada_w[layer] + ada_b[layer]
        shift1, scale1, gate1, shift2, scale2, gate2 = [m[:, None, :] for m in jnp.split(mod, 6, axis=-1)]
        h = rmsnorm(x, norm1_g[layer]) * (1.0 + scale1) + shift1
        j = layer // N_MIXERS
        if layer % N_MIXERS == 0:
            mix = dilated_mixer(h, positions, a_w_in[j], a_q_gain[j], a_k_gain[j], a_w_out[j])
        else:
            mix = gla_mixer(h, b_w_in[j], b_w_gate_up[j], b_gate_bias[j], b_out_gain[j], b_w_out[j])
        x = x + gate1 * mix
        h = rmsnorm(x, norm2_g[layer]) * (1.0 + scale2) + shift2
        x = x + gate2 * moe_ffn(h, router_w[layer], router_b[layer], moe_w_gu[layer],
                                moe_b_gu[layer], moe_w_down[layer], moe_b_down[layer])
    return x
```

```python
import numpy as np
from contextlib import ExitStack
import concourse.bass as bass
import concourse.mybir as mybir
from concourse.bass_utils import run_bass_kernel_spmd

F32 = mybir.dt.float32
BF16 = mybir.dt.bfloat16
I32 = mybir.dt.int32
U32 = mybir.dt.uint32
AF = mybir.ActivationFunctionType
ALU = mybir.AluOpType
AX = mybir.AxisListType

D = 1024
S = 4096
NT = S // 128
NE = 32
SB = 512
NSB = 63
NSLOT = NSB * SB
EPS = 1e-6
ALPHA = 1.702
EPOCH = 20000
NEGM = -30000.0
DBG_LAYER = 1


class Res:
    __slots__ = ("name", "lw", "rd")

    def __init__(self, name=""):
        self.name = name
        self.lw = None
        self.rd = []


class Buf:
    def __init__(self, t, name):
        self.t = t
        self.name = name
        self.res = {}

    def r(self, key=0):
        x = self.res.get(key)
        if x is None:
            x = Res(f"{self.name}:{key}")
            self.res[key] = x
        return x

    def __getitem__(self, idx):
        return self.t[idx]


class K:
    def __init__(self, nc, es):
        self.nc = nc
        self.es = es
        self.eng = {"pe": nc.tensor, "dve": nc.vector, "act": nc.scalar,
                    "pool": nc.gpsimd, "sp": nc.sync}
        self.esem, self.ecnt, self.eepoch = {}, {}, {}
        self.allsems = []
        for e in self.eng:
            self.eepoch[e] = 0
            self.esem[e] = self._newsem(f"p_{e}_0")
            self.ecnt[e] = 0
        self.seen = {e: {} for e in self.eng}
        self.dsem, self.dcnt, self.dnext = {}, {}, {}
        for q in ("sp", "pool"):
            n = 16
            self.dsem[q] = [self._newsem(f"d_{q}_{i}") for i in range(n)]
            self.dcnt[q] = [0] * n
            self.dnext[q] = 0
        self.nbuf = 0
        self.ninst = 0
        self.noself = {"pe"}
        self.last = {}

    def _newsem(self, name):
        s = self.es_root().enter_context(self.nc.semaphore(name))
        self.allsems.append(s)
        return s

    def es_root(self):
        return self._root if hasattr(self, "_root") else self.es

    def sb(self, name, shape, dt):
        self.nbuf += 1
        t = self.es.enter_context(self.nc.sbuf_tensor(f"{name}_{self.nbuf}", shape, dt))
        return Buf(t, name)

    def ps(self, name, shape, dt=F32):
        self.nbuf += 1
        t = self.es.enter_context(self.nc.psum_tensor(f"{name}_{self.nbuf}", shape, dt))
        return Buf(t, name)

    def _wait(self, e, sem, val):
        s = self.seen[e]
        key = id(sem)
        if s.get(key, 0) < val:
            self.eng[e].wait_ge(sem, val)
            s[key] = val

    def _deps(self, e, reads, writes):
        need = {}

        def add(tok):
            if tok is None:
                return
            sem, val = tok
            kk = id(sem)
            if kk not in need or need[kk][1] < val:
                need[kk] = (sem, val)
        for r in reads:
            add(r.lw)
        for w in writes:
            add(w.lw)
            for t in w.rd:
                add(t)
        own = id(self.esem[e])
        for sem, val in need.values():
            if e in self.noself and id(sem) == own:
                continue
            self._wait(e, sem, val)

    def _commit(self, tok, reads, writes):
        for r in reads:
            r.rd.append(tok)
            if len(r.rd) > 48:
                best = {}
                for s, v in r.rd:
                    if id(s) not in best or best[id(s)][1] < v:
                        best[id(s)] = (s, v)
                r.rd = list(best.values())
        for w in writes:
            w.lw = tok
            w.rd = []

    @staticmethod
    def _norm(lst):
        return [x.r() if isinstance(x, Buf) else x for x in lst]

    def op(self, e, fn, reads=(), writes=()):
        reads, writes = self._norm(reads), self._norm(writes)
        if self.ecnt[e] >= EPOCH:
            self.eepoch[e] += 1
            self.esem[e] = self._newsem(f"p_{e}_{self.eepoch[e]}")
            self.ecnt[e] = 0
        self._deps(e, reads, writes)
        inst = fn(self.eng[e])
        self.ecnt[e] += 1
        self.ninst += 1
        inst.then_inc(self.esem[e], 1)
        tok = (self.esem[e], self.ecnt[e])
        self.last[e] = tok
        self._commit(tok, reads, writes)
        return tok

    def dma(self, q, fn, reads=(), writes=()):
        reads, writes = self._norm(reads), self._norm(writes)
        i = self.dnext[q]
        self.dnext[q] = (i + 1) % len(self.dsem[q])
        sem = self.dsem[q][i]
        if self.dcnt[q][i] > 0:
            self._wait(q, sem, 16 * self.dcnt[q][i])
        self._deps(q, reads, writes)
        inst = fn(self.eng[q])
        self.dcnt[q][i] += 1
        self.ninst += 1
        inst.then_inc(sem, 16)
        tok = (sem, 16 * self.dcnt[q][i])
        self._commit(tok, reads, writes)
        return tok

    def barrier(self):
        toks = list(self.last.values())
        for q in self.dsem:
            for i, s in enumerate(self.dsem[q]):
                if self.dcnt[q][i]:
                    toks.append((s, 16 * self.dcnt[q][i]))
        for e in self.eng:
            for sem, val in toks:
                self._wait(e, sem, val)


def build_program(debug=False):
    nc = bass.Bass("TRN2", target_bir_lowering=False)

    def din(name, shape, dt=F32):
        return nc.dram_tensor(name, shape, dt, kind="ExternalInput")

    def dscr(name, shape, dt=F32):
        return Buf(nc.dram_tensor(name, shape, dt, kind="Internal"), name)

    x_in = din("x", [S, D])
    c_in = din("c", [1, D])
    pos_in = din("pos", [1, S], I32)
    ada_w = din("ada_w", [2, D, 6 * D])
    ada_b = din("ada_b", [2, 6 * D])
    norm1_g = din("norm1_g", [2, D])
    norm2_g = din("norm2_g", [2, D])
    a_w_in = din("a_w_in", [D, 9216])
    a_q_gain = din("a_q_gain", [3, 64])
    a_k_gain = din("a_k_gain", [3, 64])
    a_w_out = din("a_w_out", [D, D])
    b_w_in = din("b_w_in", [D, 3088])
    b_w_gate_up = din("b_w_gate_up", [16, 512])
    b_gate_bias = din("b_gate_bias", [1, 512])
    b_out_gain = din("b_out_gain", [1, 256])
    b_w_out = din("b_w_out", [D, D])
    router_w = din("router_w", [2, D, NE])
    router_b = din("router_b", [2, NE])
    moe_w_gu = din("moe_w_gu", [2, NE * D, 2 * D])
    moe_b_gu = din("moe_b_gu", [2, NE, 2 * D])
    moe_w_down = din("moe_w_down", [2, NE * D, D])
    moe_b_down = din("moe_b_down", [2, NE, D])
    cst = din("cst", [128, 1024])
    cst2 = din("cst2", [128, 384])
    out = nc.dram_tensor("out", [S, D], F32, kind="ExternalOutput")
    outb = Buf(out, "out")
    dbg = Buf(nc.dram_tensor("dbg", [S, D], F32, kind="ExternalOutput"), "dbg") if debug else None

    modbc = dscr("modbc", [2, 6, 128, D])
    xs1 = dscr("xs1", [S, D])
    xs2 = dscr("xs2", [S, D])
    Xs = dscr("Xs", [NSLOT, D], BF16)
    Ys = dscr("Ys", [NSLOT, D])
    OT = dscr("OT", [D, S], BF16)

    with ExitStack() as root:
        k = K.__new__(K)
        k._root = root
        K.__init__(k, nc, root)

        cf = k.sb("cf", [128, 1024], F32)
        k.dma("sp", lambda e: e.dma_start(out=cf[:], in_=cst.ap()), writes=[cf])
        ident_f = cf[:, 0:128]
        Uex = cf[:, 128:256]
        iota_e = cf[:, 256:288]
        thr8 = cf[:, 288:296]
        jidx = cf[:, 296:296 + NSB]
        pidx = cf[:, 360:368]
        iota_p = cf[:, 368:369]
        invf = cf[:, 369:370]
        cb = k.sb("cb", [128, 640], BF16)
        k.op("dve", lambda e: e.tensor_copy(cb[:], cf[:, 384:1024]), reads=[cf], writes=[cb])
        RT = cb[:, 0:128]
        blk = cb[:, 128:256]
        mcur = cb[:, 256:384]
        mprev = cb[:, 384:512]
        ident_b = cb[:, 512:640]
        cf2 = k.sb("cf2", [128, 384], F32)
        k.dma("sp", lambda e: e.dma_start(out=cf2[:], in_=cst2.ap()), writes=[cf2])
        cb2 = k.sb("cb2", [128, 384], BF16)
        k.op("dve", lambda e: e.tensor_copy(cb2[:], cf2[:]), reads=[cf2], writes=[cb2])
        gmask = cb2[:, 0:128]
        mask2 = cb2[:, 128:384].rearrange("p (a b) -> p a b", a=2)
        ones_f = k.sb("ones_f", [128, 128], F32)
        k.op("dve", lambda e: e.memset(ones_f[:], 1.0), writes=[ones_f])
        ones_b = k.sb("ones_b", [128, 512], BF16)
        k.op("dve", lambda e: e.memset(ones_b[:], 1.0), writes=[ones_b])
        PERS = (k.sb("widx", [128, NSB, 8], I32), k.sb("ejb", [128, NSB], F32),
                k.sb("dkp", [128, NT, 4], I32), k.sb("gkp", [128, NT, 4], F32))

        with ExitStack() as ph:
            k.es = ph
            cc = k.sb("cc", [128, 8], F32)
            with nc.allow_non_contiguous_dma(reason="tiny c load"):
                k.dma("sp", lambda e: e.dma_start(out=cc[:], in_=c_in.ap().rearrange("o (kc p) -> p (o kc)", p=128)), writes=[cc])
            k.op("act", lambda e: e.activation(cc[:], cc[:], AF.Silu), reads=[cc], writes=[cc])
            crep = k.sb("crep", [128, 8, 128], F32)
            k.op("dve", lambda e: e.tensor_copy(crep[:], cc[:].unsqueeze(2).to_broadcast([128, 8, 128])), reads=[cc], writes=[crep])
            wa = [k.sb("wa", [128, 3072], F32) for _ in range(2)]
            brow = k.sb("brow", [1, 6 * D], F32)
            pm = [k.ps("pm", [128, 512]) for _ in range(6)]
            stg = [k.sb("stg", [128, 512], F32) for _ in range(2)]
            it = 0
            for l in range(2):
                k.dma("sp", lambda e, l=l: e.dma_start(out=brow[:], in_=ada_b.ap()[l:l + 1, :]), writes=[brow])
                for half in range(2):
                    for kc in range(8):
                        w = wa[it % 2]
                        it += 1
                        k.dma("sp", lambda e, w=w, l=l, kc=kc, half=half: e.dma_start(
                            out=w[:], in_=ada_w.ap()[l, kc * 128:(kc + 1) * 128, half * 3072:(half + 1) * 3072]), writes=[w])
                        for g in range(6):
                            k.op("pe", lambda e, w=w, g=g, kc=kc: e.matmul(pm[g][:], lhsT=crep[:, kc, :], rhs=w[:, g * 512:(g + 1) * 512],
                                                                           start=(kc == 0), stop=False), reads=[w, crep], writes=[pm[g]])
                    for g in range(6):
                        col = half * 3072 + g * 512
                        k.op("pe", lambda e, g=g, col=col: e.matmul(pm[g][:], lhsT=ones_f[0:1, :], rhs=brow[0:1, col:col + 512],
                                                                    start=False, stop=True), reads=[brow, ones_f], writes=[pm[g]])
                        st = stg[g % 2]
                        k.op("act", lambda e, st=st, g=g: e.activation(st[:], pm[g][:], AF.Identity), reads=[pm[g]], writes=[st])
                        slot, hf = col // 1024, (col % 1024) // 512
                        k.dma("sp", lambda e, st=st, l=l, slot=slot, hf=hf: e.dma_start(
                            out=modbc[l, slot, :, hf * 512:(hf + 1) * 512], in_=st[:]), reads=[st], writes=[modbc.r((l, slot))])
            k.barrier()

        def load_mod(l, which, normg):
            SH = k.sb("SH", [128, D], F32)
            G = k.sb("G", [128, D], F32)
            gt = k.sb("gt", [128, D], F32)
            b = 3 * which
            k.dma("sp", lambda e: e.dma_start(out=SH[:], in_=modbc[l, b + 0]), reads=[modbc.r((l, b))], writes=[SH])
            k.dma("sp", lambda e: e.dma_start(out=G[:], in_=modbc[l, b + 1]), reads=[modbc.r((l, b + 1))], writes=[G])
            k.dma("sp", lambda e: e.dma_start(out=gt[:], in_=normg.ap()[l:l + 1, :].partition_broadcast(128)), writes=[gt])
            k.op("dve", lambda e: e.scalar_tensor_tensor(G[:], G[:], 1.0, gt[:], ALU.add, ALU.mult), reads=[G, gt], writes=[G])
            return G, SH

        def load_gate(l, which):
            GT = k.sb("GT", [128, D], F32)
            k.dma("sp", lambda e: e.dma_start(out=GT[:], in_=modbc[l, 3 * which + 2]), reads=[modbc.r((l, 3 * which + 2))], writes=[GT])
            return GT

        class NormCtx:
            pass

        def make_norm():
            n = NormCtx()
            n.xt = [k.sb("n_xt", [128, D], F32) for _ in range(2)]
            n.junk = k.sb("n_junk", [128, D], BF16)
            n.ss = [k.sb("n_ss", [128, 4], F32) for _ in range(2)]
            n.h = [k.sb("n_h", [128, D], F32) for _ in range(2)]
            n.pt = [k.ps("n_pt", [128, 512]) for _ in range(2)]
            n.i = 0
            return n

        def norm_tile(n, src, srcres, t, G, SH):
            i = n.i
            n.i += 1
            xt, ss, h = n.xt[i % 2], n.ss[i % 2], n.h[i % 2]
            k.dma("sp", lambda e: e.dma_start(out=xt[:], in_=src[t * 128:(t + 1) * 128, :]), reads=srcres, writes=[xt])
            k.op("act", lambda e: e.activation(n.junk[:], xt[:], AF.Square, accum_out=ss[:, 0:1]), reads=[xt], writes=[n.junk, ss])
            k.op("dve", lambda e: e.tensor_scalar(ss[:, 1:2], ss[:, 0:1], 1.0 / D, EPS, ALU.mult, ALU.add), reads=[ss], writes=[ss])
            k.op("act", lambda e: e.activation(ss[:, 2:3], ss[:, 1:2], AF.Ln), reads=[ss], writes=[ss])
            k.op("act", lambda e: e.activation(ss[:, 3:4], ss[:, 2:3], AF.Exp, scale=-0.5), reads=[ss], writes=[ss])
            k.op("dve", lambda e: e.scalar_tensor_tensor(h[:], xt[:], ss[:, 3:4], G[:], ALU.mult, ALU.mult), reads=[xt, ss, G], writes=[h])
            k.op("dve", lambda e: e.tensor_tensor(h[:], h[:], SH[:], ALU.add), reads=[h, SH], writes=[h])
            return xt, h

        def transpose_to(n, h, dsts):
            for half in range(2):
                pt = n.pt[half]
                for j in range(4):
                    kc = half * 4 + j
                    k.op("pe", lambda e, pt=pt, j=j, kc=kc: e.transpose(pt[:, j * 128:(j + 1) * 128], h[:, kc * 128:(kc + 1) * 128], ident_f),
                         reads=[h, cf], writes=[pt])
                for (fn, res, eng) in dsts:
                    o = fn(half)
                    if eng == "act":
                        k.op("act", lambda e, o=o, pt=pt: e.activation(o, pt[:].rearrange("p (a b) -> p a b", a=4), AF.Identity), reads=[pt], writes=res)
                    else:
                        k.op("dve", lambda e, o=o, pt=pt: e.tensor_copy(o, pt[:].rearrange("p (a b) -> p a b", a=4)), reads=[pt], writes=res)

        def moe_layer(l, xsrc, xdst):
            widx, ejb, dkp, gkp = PERS
            with ExitStack() as ph:
                k.es = ph
                G, SH = load_mod(l, 1, norm2_g)
                n = make_norm()
                rw = k.sb("rw", [128, 8, NE], F32)
                k.dma("sp", lambda e: e.dma_start(out=rw[:], in_=router_w.ap()[l].rearrange("(kc p) e -> p kc e", p=128)), writes=[rw])
                rb = k.sb("rb", [1, NE], F32)
                k.dma("sp", lambda e: e.dma_start(out=rb[:], in_=router_b.ap()[l:l + 1, :]), writes=[rb])
                h2b = k.sb("h2b", [128, NT, D], BF16)
                hT32 = [k.sb("hT32", [128, 8, 128], F32) for _ in range(2)]
                pl = [k.ps("pl", [128, NE]) for _ in range(2)]
                lg = [k.sb("lg", [128, NE], F32) for _ in range(2)]
                m8 = [k.sb("m8", [128, 8], F32) for _ in range(2)]
                ex = [k.sb("ex", [128, NE], F32) for _ in range(2)]
                sm = [k.sb("sm", [128, 4], F32) for _ in range(2)]
                Mall = k.sb("Mall", [128, NT, NE], F32)
                GWall = k.sb("GWall", [128, NT, NE], F32)
                idx8 = k.sb("idx8", [128, NT, 8], U32)
                def rt_a(t):
                    xt, h = norm_tile(n, xsrc, [xsrc.r(t)], t, G, SH)
                    k.op("act", lambda e: e.activation(h2b[:, t, :], h[:], AF.Identity), reads=[h], writes=[h2b.r(t)])
                    hT = hT32[t % 2]
                    transpose_to(n, h, [(lambda half: hT[:, half * 4:(half + 1) * 4, :], [hT.r(0)], "dve")])
                    p = pl[t % 2]
                    for kc in range(8):
                        k.op("pe", lambda e, kc=kc: e.matmul(p[:], lhsT=hT[:, kc, :], rhs=rw[:, kc, :], start=(kc == 0), stop=False),
                             reads=[hT.r(0), rw], writes=[p])
                    k.op("pe", lambda e: e.matmul(p[:], lhsT=ones_f[0:1, :], rhs=rb[0:1, :], start=False, stop=True), reads=[rb, ones_f], writes=[p])

                def rt_b(t):
                    p = pl[t % 2]
                    L, M8, E, SM = lg[t % 2], m8[t % 2], ex[t % 2], sm[t % 2]
                    k.op("dve", lambda e: e.tensor_copy(L[:], p[:]), reads=[p], writes=[L])
                    k.op("dve", lambda e: e.max(M8[:], L[:]), reads=[L], writes=[M8])
                    k.op("dve", lambda e: e.max_index(idx8[:, t, :], M8[:], L[:]), reads=[L, M8], writes=[idx8.r(t)])
                    k.op("dve", lambda e: e.tensor_scalar(Mall[:, t, :], L[:], M8[:, 3:4], None, ALU.is_ge), reads=[L, M8], writes=[Mall.r(t)])
                    k.op("dve", lambda e: e.tensor_scalar(SM[:, 0:1], M8[:, 0:1], -1.0, None, ALU.mult), reads=[M8], writes=[SM])
                    k.op("act", lambda e: e.activation(E[:], L[:], AF.Exp, bias=SM[:, 0:1], scale=1.0), reads=[L, SM], writes=[E])
                    k.op("dve", lambda e: e.tensor_tensor(E[:], E[:], Mall[:, t, :], ALU.mult), reads=[E, Mall.r(t)], writes=[E])
                    k.op("dve", lambda e: e.tensor_reduce(SM[:, 1:2], E[:], AX.X, ALU.add), reads=[E], writes=[SM])
                    k.op("dve", lambda e: e.reciprocal(SM[:, 2:3], SM[:, 1:2]), reads=[SM], writes=[SM])
                    k.op("dve", lambda e: e.tensor_scalar(GWall[:, t, :], E[:], SM[:, 2:3], None, ALU.mult), reads=[E, SM], writes=[GWall.r(t)])
                for t in range(NT + 1):
                    if t < NT:
                        rt_a(t)
                    if t >= 1:
                        rt_b(t - 1)
                Mres = [Mall.r(t) for t in range(NT)]
                pp = [k.ps("pp", [128, 512]) for _ in range(2)]
                ptot = [k.ps("ptot", [128, 512]) for _ in range(2)]
                Mflat = Mall[:].rearrange("p t e -> p (t e)")
                pre = k.sb("pre", [128, NT, NE], F32)
                tot = k.sb("tot", [128, NT, NE], F32)
                for hf in range(2):
                    k.op("pe", lambda e, hf=hf: e.matmul(pp[hf][:], lhsT=Uex, rhs=Mflat[:, hf * 512:(hf + 1) * 512], start=True, stop=True), reads=Mres + [cf], writes=[pp[hf]])
                    k.op("pe", lambda e, hf=hf: e.matmul(ptot[hf][:], lhsT=ones_f[:], rhs=Mflat[:, hf * 512:(hf + 1) * 512], start=True, stop=True), reads=Mres + [ones_f], writes=[ptot[hf]])
                    k.op("dve", lambda e, hf=hf: e.tensor_copy(pre[:].rearrange("p t e -> p (t e)")[:, hf * 512:(hf + 1) * 512], pp[hf][:]), reads=[pp[hf]], writes=[pre.r(hf)])
                    k.op("dve", lambda e, hf=hf: e.tensor_copy(tot[:].rearrange("p t e -> p (t e)")[:, hf * 512:(hf + 1) * 512], ptot[hf][:]), reads=[ptot[hf]], writes=[tot.r(hf)])
                off = k.sb("off", [128, NT + 1, NE], F32)
                k.op("dve", lambda e: e.memset(off[:, 0, :], 0.0), writes=[off])
                for t in range(NT):
                    k.op("dve", lambda e, t=t: e.tensor_tensor(off[:, t + 1, :], off[:, t, :], tot[:, t, :], ALU.add), reads=[off, tot.r(0), tot.r(1)], writes=[off])
                cnt = off[:, NT, :]
                cmp8 = k.sb("cmp8", [128, NE, 8], F32)
                nb = k.sb("nb", [128, 3, NE], F32)
                k.op("dve", lambda e: e.tensor_tensor(cmp8[:], cnt.unsqueeze(2).to_broadcast([128, NE, 8]), thr8.unsqueeze(1).to_broadcast([128, NE, 8]), ALU.is_gt),
                     reads=[off, cf], writes=[cmp8])
                k.op("dve", lambda e: e.tensor_reduce(nb[:, 0, :], cmp8[:], AX.X, ALU.add), reads=[cmp8], writes=[nb])
                k.op("dve", lambda e: e.tensor_tensor_scan(nb[:, 1, :], ones_f[:, 0:NE], nb[:, 0, :], 0.0, ALU.mult, ALU.add), reads=[nb, ones_f], writes=[nb])
                k.op("dve", lambda e: e.tensor_tensor(nb[:, 2, :], nb[:, 1, :], nb[:, 0, :], ALU.subtract), reads=[nb], writes=[nb])
                k.op("dve", lambda e: e.tensor_scalar(nb[:, 2, :], nb[:, 2, :], float(SB), None, ALU.mult), reads=[nb], writes=[nb])
                cmpj = k.sb("cmpj", [128, NSB, NE], F32)
                ej = k.sb("ej", [128, NSB], F32)
                k.op("dve", lambda e: e.tensor_tensor(cmpj[:], nb[:, 1, :].unsqueeze(1).to_broadcast([128, NSB, NE]), jidx.unsqueeze(2).to_broadcast([128, NSB, NE]), ALU.is_le),
                     reads=[nb, cf], writes=[cmpj])
                k.op("dve", lambda e: e.tensor_reduce(ej[:], cmpj[:], AX.X, ALU.add), reads=[cmpj], writes=[ej])
                k.op("dve", lambda e: e.tensor_scalar(ej[:], ej[:], float(NE - 1), None, ALU.min), reads=[ej], writes=[ej])
                k.op("dve", lambda e: e.tensor_tensor(pre[:], pre[:], off[:, 0:NT, :], ALU.add), reads=[pre.r(0), pre.r(1), off], writes=[pre.r(0), pre.r(1)])
                k.op("dve", lambda e: e.tensor_tensor(pre[:], pre[:], nb[:, 2, :].unsqueeze(1).to_broadcast([128, NT, NE]), ALU.add), reads=[pre.r(0), pre.r(1), nb], writes=[pre.r(0), pre.r(1)])
                idxf = k.sb("idxf", [128, NT, 8], F32)
                k.op("dve", lambda e: e.tensor_copy(idxf[:], idx8[:]), reads=[idx8.r(t) for t in range(NT)], writes=[idxf])
                oh = k.sb("oh", [128, NT, 4, NE], F32)
                prod = k.sb("prod", [128, NT, 4, NE], F32)
                dk = k.sb("dk", [128, NT, 4], F32)
                gk = k.sb("gk", [128, NT, 4], F32)
                GWres = [GWall.r(t) for t in range(NT)]
                k.op("dve", lambda e: e.tensor_tensor(oh[:], iota_e.unsqueeze(1).unsqueeze(1).to_broadcast([128, NT, 4, NE]),
                                                      idxf[:, :, 0:4].unsqueeze(3).to_broadcast([128, NT, 4, NE]), ALU.is_equal), reads=[idxf, cf], writes=[oh])
                k.op("dve", lambda e: e.tensor_tensor(prod[:], oh[:], pre[:].unsqueeze(2).to_broadcast([128, NT, 4, NE]), ALU.mult), reads=[oh, pre.r(0), pre.r(1)], writes=[prod])
                k.op("dve", lambda e: e.tensor_reduce(dk[:].rearrange("p t k -> p (t k)"), prod[:].rearrange("p t k e -> p (t k) e"), AX.X, ALU.add), reads=[prod], writes=[dk])
                k.op("dve", lambda e: e.tensor_tensor(prod[:], oh[:], GWall[:].unsqueeze(2).to_broadcast([128, NT, 4, NE]), ALU.mult), reads=[oh] + GWres, writes=[prod])
                k.op("dve", lambda e: e.tensor_reduce(gk[:].rearrange("p t k -> p (t k)"), prod[:].rearrange("p t k e -> p (t k) e"), AX.X, ALU.add), reads=[prod], writes=[gk])
                dki = k.sb("dki", [128, NT, 4], I32)
                k.op("dve", lambda e: e.tensor_copy(dki[:], dk[:]), reads=[dk], writes=[dki])
                widxf = k.sb("widxf", [128, NSB, 8], F32)
                k.op("dve", lambda e: e.scalar_tensor_tensor(widxf[:], ej[:].unsqueeze(2).to_broadcast([128, NSB, 8]), 1024.0, pidx.unsqueeze(1).to_broadcast([128, NSB, 8]), ALU.mult, ALU.add),
                     reads=[ej, cf], writes=[widxf])
                widx, ejb, dkp, gkp = PERS
                k.op("dve", lambda e: e.tensor_scalar(widxf[:], widxf[:], float(l * NE * D), None, ALU.add), reads=[widxf], writes=[widxf])
                k.op("dve", lambda e: e.tensor_copy(widx[:], widxf[:]), reads=[widxf], writes=[widx])
                k.op("dve", lambda e: e.tensor_copy(ejb[:], ej[:]), reads=[ej], writes=[ejb])
                k.op("dve", lambda e: e.tensor_copy(dkp[:], dki[:]), reads=[dki], writes=[dkp])
                k.op("dve", lambda e: e.tensor_copy(gkp[:], gk[:]), reads=[gk], writes=[gkp])
                for t in range(NT):
                    for kk in range(4):
                        k.dma("pool", lambda e, t=t, kk=kk: e.indirect_dma_start(
                            out=Xs[:, :], out_offset=bass.IndirectOffsetOnAxis(ap=dkp[:, t, kk:kk + 1], axis=0),
                            in_=h2b[:, t, :], in_offset=None), reads=[h2b.r(t), dkp], writes=[Xs.r((t, kk))])
                k.barrier()

            with ExitStack() as ph:
                k.es = ph
                Wgu = [k.sb("Wgu", [128, 8, 2 * D], BF16) for _ in range(2)]
                Wd = [k.sb("Wd", [128, 8, D], BF16) for _ in range(2)]
                Bd = k.sb("Bd", [NE, D], BF16)
                Bg32 = k.sb("Bg32", [NE, 2 * D], F32)
                k.dma("sp", lambda e: e.dma_start(out=Bg32[:], in_=moe_b_gu.ap()[l]), writes=[Bg32])
                BguT = k.sb("BguT", [128, 16, NE], F32)
                c78 = k.sb("c78", [128, 2], F32)
                k.op("dve", lambda e: e.memset(c78[:, 0:1], 7.0), writes=[c78])
                k.op("dve", lambda e: e.memset(c78[:, 1:2], 8.0), reads=[c78], writes=[c78])
                ohf = [k.sb("ohf", [128, NE], F32) for _ in range(2)]
                bprod = [k.sb("bprod", [128, 16, NE], F32) for _ in range(2)]
                bcol = [k.sb("bcol", [128, 16], F32) for _ in range(2)]
                k.dma("pool", lambda e: e.dma_start(out=Bd[:], in_=moe_b_down.ap()[l]), writes=[Bd])
                Xt = [k.sb("Xt", [128, 4, D], BF16) for _ in range(2)]
                XT = [k.sb("XT", [128, 8, SB], BF16) for _ in range(2)]
                AT = [k.sb("AT", [128, 8, SB], BF16) for _ in range(2)]
                OHD = [k.sb("OHD", [NE, 128], BF16) for _ in range(2)]
                gm = [k.sb("gm", [128, SB], F32) for _ in range(2)]
                gs = [k.sb("gs", [128, SB], F32) for _ in range(2)]
                um = [k.sb("um", [128, SB], F32) for _ in range(2)]
                Yt = [k.sb("Yt", [128, D], F32) for _ in range(2)]
                ptr = [k.ps("ptr", [128, 4, 128], BF16) for _ in range(2)]
                pg = [k.ps("pg", [128, SB]) for _ in range(2)]
                pu = [k.ps("pu", [128, SB]) for _ in range(2)]
                pd = [k.ps("pd", [128, 512]) for _ in range(2)]
                for fc in range(16):
                    k.op("pe", lambda e, fc=fc: e.transpose(pd[0][:, fc * NE:(fc + 1) * NE], Bg32[:, fc * 128:(fc + 1) * 128], ident_f[0:NE, 0:NE]), reads=[Bg32, cf], writes=[pd[0]])
                k.op("dve", lambda e: e.tensor_copy(BguT[:].rearrange("p a b -> p (a b)"), pd[0][:]), reads=[pd[0]], writes=[BguT])
                cnt_ = {"ci": 0, "yi": 0}
                wgu_flat = moe_w_gu.ap().rearrange("l r n -> (l r) n")
                wd_flat = moe_w_down.ap().rearrange("l r n -> (l r) n")

                def stage_load(j):
                    wg, wd, xt, ohd = Wgu[j % 2], Wd[j % 2], Xt[j % 2], OHD[j % 2]
                    k.dma("sp", lambda e: e.dma_start(out=xt[:], in_=Xs[j * SB:(j + 1) * SB, :].rearrange("(a p) d -> p a d", p=128)), reads=[], writes=[xt])
                    for kc in range(8):
                        k.dma("pool", lambda e, kc=kc: e.indirect_dma_start(
                            out=wg[:, kc, :], out_offset=None, in_=wgu_flat,
                            in_offset=bass.IndirectOffsetOnAxis(ap=widx[:, j, kc:kc + 1], axis=0)), reads=[widx], writes=[wg.r(kc)])
                    for kc in range(8):
                        k.dma("pool", lambda e, kc=kc: e.indirect_dma_start(
                            out=wd[:, kc, :], out_offset=None, in_=wd_flat,
                            in_offset=bass.IndirectOffsetOnAxis(ap=widx[:, j, kc:kc + 1], axis=0)), reads=[widx], writes=[wd.r(kc)])
                    OF_, BP_, BC_ = ohf[j % 2], bprod[j % 2], bcol[j % 2]
                    k.op("dve", lambda e: e.tensor_scalar(OF_[:], iota_e, ejb[:, j:j + 1], None, ALU.is_equal), reads=[ejb, cf], writes=[OF_])
                    k.op("dve", lambda e: e.tensor_tensor(BP_[:], BguT[:], OF_[:].unsqueeze(1).to_broadcast([128, 16, NE]), ALU.mult), reads=[BguT, OF_], writes=[BP_])
                    k.op("dve", lambda e: e.tensor_reduce(BC_[:], BP_[:], AX.X, ALU.add), reads=[BP_], writes=[BC_])
                    k.op("dve", lambda e: e.tensor_scalar(BC_[:, 8:16], BC_[:, 8:16], 1.0, None, ALU.add), reads=[BC_], writes=[BC_])
                    k.op("dve", lambda e: e.tensor_scalar(ohd[:], ejb[0:NE, j:j + 1].to_broadcast([NE, 128]), iota_p[0:NE, :], ALPHA, ALU.is_equal, ALU.mult),
                         reads=[ejb, cf], writes=[ohd])

                def stage_T(j):
                    xt, xT = Xt[j % 2], XT[j % 2]
                    for kc in range(8):
                        pt = ptr[kc % 2]
                        for a in range(4):
                            k.op("pe", lambda e, pt=pt, a=a, kc=kc: e.transpose(pt[:, a, :], xt[:, a, kc * 128:(kc + 1) * 128], ident_b), reads=[xt, cb], writes=[pt])
                        if kc % 2 == 0:
                            k.op("act", lambda e, pt=pt, kc=kc: e.activation(xT[:, kc, :], pt[:].rearrange("p a b -> p (a b)"), AF.Identity), reads=[pt], writes=[xT.r(kc)])
                        else:
                            k.op("dve", lambda e, pt=pt, kc=kc: e.tensor_copy(xT[:, kc, :], pt[:].rearrange("p a b -> p (a b)")), reads=[pt], writes=[xT.r(kc)])

                def stage_gu(j):
                    wg, xT, aT = Wgu[j % 2], XT[j % 2], AT[j % 2]
                    BC_ = bcol[j % 2]
                    for gc in range(8):
                        ci = cnt_["ci"]
                        cnt_["ci"] += 1
                        PG, PU = pg[ci % 2], pu[ci % 2]
                        GM, GS, UM = gm[ci % 2], gs[ci % 2], um[ci % 2]
                        for (P_, fc) in ((PG, gc), (PU, gc + 8)):
                            for kc in range(8):
                                k.op("pe", lambda e, P_=P_, fc=fc, kc=kc: e.matmul(P_[:], lhsT=wg[:, kc, fc * 128:(fc + 1) * 128], rhs=xT[:, kc, :],
                                                                           start=(kc == 0), stop=(kc == 7)), reads=[wg.r(kc), xT.r(kc)], writes=[P_])
                        k.op("dve", lambda e, GM=GM, PG=PG, gc=gc: e.tensor_scalar(GM[:], PG[:], BC_[:, gc:gc + 1], c78[:, 0:1], ALU.add, ALU.min), reads=[PG, BC_, c78], writes=[GM])
                        k.op("act", lambda e, GM=GM, GS=GS: e.activation(GS[:], GM[:], AF.Silu, scale=ALPHA), reads=[GM], writes=[GS])
                        k.op("dve", lambda e, UM=UM, PU=PU, gc=gc: e.tensor_scalar(UM[:], PU[:], BC_[:, 8 + gc:9 + gc], c78[:, 1:2], ALU.add, ALU.min), reads=[PU, BC_, c78], writes=[UM])
                        k.op("dve", lambda e, UM=UM, GS=GS, gc=gc: e.scalar_tensor_tensor(aT[:, gc, :], UM[:], -6.0, GS[:], ALU.max, ALU.mult),
                             reads=[UM, GS], writes=[aT.r(gc)])

                def stage_down(j):
                    wd, aT, ohd = Wd[j % 2], AT[j % 2], OHD[j % 2]
                    for a in range(4):
                        yi = cnt_["yi"]
                        cnt_["yi"] += 1
                        Y = Yt[yi % 2]
                        for nh in range(2):
                            PD = pd[nh]
                            for gc in range(8):
                                k.op("pe", lambda e, PD=PD, gc=gc, a=a, nh=nh: e.matmul(PD[:], lhsT=aT[:, gc, a * 128:(a + 1) * 128], rhs=wd[:, gc, nh * 512:(nh + 1) * 512],
                                                                                start=(gc == 0), stop=False), reads=[aT.r(gc), wd.r(gc)], writes=[PD])
                            k.op("pe", lambda e, PD=PD, nh=nh: e.matmul(PD[:], lhsT=ohd[:], rhs=Bd[:, nh * 512:(nh + 1) * 512], start=False, stop=True),
                                 reads=[ohd, Bd], writes=[PD])
                            k.op("act", lambda e, PD=PD, Y=Y, nh=nh: e.activation(Y[:, nh * 512:(nh + 1) * 512], PD[:], AF.Identity, scale=1.0 / ALPHA), reads=[PD], writes=[Y.r(nh)])
                        k.dma("sp", lambda e, Y=Y, a=a: e.dma_start(out=Ys[j * SB + a * 128: j * SB + (a + 1) * 128, :], in_=Y[:]), reads=[Y.r(0), Y.r(1)], writes=[Ys.r((j, a))])

                stage_load(0)
                stage_T(0)
                for j in range(NSB):
                    if j + 1 < NSB:
                        stage_load(j + 1)
                    stage_gu(j)
                    if j + 1 < NSB:
                        stage_T(j + 1)
                    stage_down(j)
                k.barrier()

            with ExitStack() as ph:
                k.es = ph
                GT = load_gate(l, 1)
                xr = [k.sb("xr", [128, D], F32) for _ in range(2)]
                yk = [k.sb("yk", [128, D], F32) for _ in range(8)]
                acc = [k.sb("acc", [128, D], F32) for _ in range(2)]
                toks = []
                for t in range(NT):
                    X_, A_ = xr[t % 2], acc[t % 2]
                    k.dma("sp", lambda e, X_=X_, t=t: e.dma_start(out=X_[:], in_=xsrc[t * 128:(t + 1) * 128, :]), reads=[xsrc.r(t)], writes=[X_])
                    for kk in range(4):
                        Yk = yk[(t % 2) * 4 + kk]
                        k.dma("pool", lambda e, Yk=Yk, t=t, kk=kk: e.indirect_dma_start(
                            out=Yk[:], out_offset=None, in_=Ys[:, :], in_offset=bass.IndirectOffsetOnAxis(ap=dkp[:, t, kk:kk + 1], axis=0)),
                            reads=[dkp], writes=[Yk])
                        if kk == 0:
                            k.op("dve", lambda e, A_=A_, Yk=Yk, t=t, kk=kk: e.tensor_scalar(A_[:], Yk[:], gkp[:, t, kk:kk + 1], None, ALU.mult), reads=[Yk, gkp], writes=[A_])
                        else:
                            k.op("dve", lambda e, A_=A_, Yk=Yk, t=t, kk=kk: e.scalar_tensor_tensor(A_[:], Yk[:], gkp[:, t, kk:kk + 1], A_[:], ALU.mult, ALU.add), reads=[Yk, gkp, A_], writes=[A_])
                    k.op("dve", lambda e, A_=A_: e.tensor_tensor(A_[:], A_[:], GT[:], ALU.mult), reads=[A_, GT], writes=[A_])
                    k.op("dve", lambda e, A_=A_, X_=X_: e.tensor_tensor(A_[:], A_[:], X_[:], ALU.add), reads=[A_, X_], writes=[A_])
                    toks.append(k.dma("sp", lambda e, A_=A_, t=t: e.dma_start(out=xdst[t * 128:(t + 1) * 128, :], in_=A_[:]), reads=[A_], writes=[xdst.r(t)]))
                k.barrier()
            return toks


        def build_hT(l, xsrc, hT):
            with ExitStack() as ph2:
                k.es = ph2
                G, SH = load_mod(l, 0, norm1_g)
                n = make_norm()
                for t in range(NT):
                    xt, h = norm_tile(n, xsrc, [xsrc.r(t)], t, G, SH)
                    transpose_to(n, h, [(lambda half, t=t: hT[:, half * 4:(half + 1) * 4, t * 128:(t + 1) * 128], [hT.r(t)], "act")])
                k.barrier()

        def out_proj(l, w_out, xsrc, xdst):
            with ExitStack() as ph2:
                k.es = ph2
                GT = load_gate(l, 0)
                oT = k.sb("oT", [128, 8, S], BF16)
                k.dma("sp", lambda e: e.dma_start(out=oT[:], in_=OT[:, :].rearrange("(c p) s -> p c s", p=128)), writes=[oT])
                wo = k.sb("wo", [128, 8, D], BF16)
                k.dma("pool", lambda e: e.dma_start(out=wo[:], in_=w_out.ap().rearrange("(c p) n -> p c n", p=128)), writes=[wo])
                xr = [k.sb("xr", [128, D], F32) for _ in range(2)]
                x1 = [k.sb("x1", [128, D], F32) for _ in range(2)]
                po = [k.ps("po", [128, 512]) for _ in range(4)]
                for t in range(NT):
                    X_, X1 = xr[t % 2], x1[t % 2]
                    k.dma("sp", lambda e, X_=X_, t=t: e.dma_start(out=X_[:], in_=xsrc[t * 128:(t + 1) * 128, :]), reads=[xsrc.r(t)], writes=[X_])
                    for nh in range(2):
                        P_ = po[(t % 2) * 2 + nh]
                        for c_ in range(8):
                            k.op("pe", lambda e, P_=P_, c_=c_, t=t, nh=nh: e.matmul(P_[:], lhsT=oT[:, c_, t * 128:(t + 1) * 128], rhs=wo[:, c_, nh * 512:(nh + 1) * 512],
                                                                            start=(c_ == 0), stop=(c_ == 7)), reads=[oT, wo], writes=[P_])
                        k.op("dve", lambda e, P_=P_, X1=X1, nh=nh: e.tensor_tensor(X1[:, nh * 512:(nh + 1) * 512], P_[:], GT[:, nh * 512:(nh + 1) * 512], ALU.mult),
                             reads=[P_, GT], writes=[X1.r(nh)])
                        k.op("dve", lambda e, X_=X_, X1=X1, nh=nh: e.tensor_tensor(X1[:, nh * 512:(nh + 1) * 512], X1[:, nh * 512:(nh + 1) * 512], X_[:, nh * 512:(nh + 1) * 512], ALU.add),
                             reads=[X1.r(nh), X_], writes=[X1.r(nh)])
                    k.dma("sp", lambda e, X1=X1, t=t: e.dma_start(out=xdst[t * 128:(t + 1) * 128, :], in_=X1[:]), reads=[X1.r(0), X1.r(1)], writes=[xdst.r(t)])
                    if dbg is not None and l == DBG_LAYER:
                        k.dma("sp", lambda e, X1=X1, t=t: e.dma_start(out=dbg[t * 128:(t + 1) * 128, :], in_=X1[:]), reads=[X1.r(0), X1.r(1)], writes=[dbg.r(t)])
                k.barrier()

        def attn_layer(l, xsrc, xdst):
            with ExitStack() as ph:
                k.es = ph
                hT = k.sb("hT", [128, 8, S], BF16)
                build_hT(l, xsrc, hT)
                k.es = ph
                hTres = [hT.r(t) for t in range(NT)]
                Ct = k.sb("Ct", [128, S], BF16)
                St = k.sb("St", [128, S], BF16)
                epsc = k.sb("epsc", [128, 1], F32)
                k.op("dve", lambda e: e.memset(epsc[:], EPS), writes=[epsc])
                gcol = k.sb("gcol", [128, 3, 2], F32)
                with nc.allow_non_contiguous_dma(reason="tiny gain loads"):
                    for s_, src in ((0, a_q_gain), (1, a_k_gain)):
                        for hf in range(2):
                            k.dma("sp", lambda e, s_=s_, src=src, hf=hf: e.dma_start(out=gcol[hf * 64:(hf + 1) * 64, :, s_], in_=src.ap().rearrange("g e -> e g")), writes=[gcol])
                with ExitStack() as ph2:
                    k.es = ph2
                    PI = float(np.pi)
                    for c4 in range(4):
                        sl = slice(c4 * 1024, (c4 + 1) * 1024)
                        pi_ = k.sb("pi_", [128, 1024], I32)
                        ang = k.sb("ang", [128, 1024], F32)
                        yy = k.sb("yy", [128, 1024], F32)
                        ni = k.sb("ni", [128, 1024], I32)
                        k.dma("sp", lambda e, pi_=pi_, sl=sl: e.dma_start(out=pi_[:], in_=pos_in.ap()[:, sl].partition_broadcast(128)), writes=[pi_])
                        k.op("dve", lambda e, ang=ang, pi_=pi_: e.tensor_copy(ang[:], pi_[:]), reads=[pi_], writes=[ang])
                        k.op("dve", lambda e, ang=ang: e.tensor_scalar(ang[:], ang[:], invf, None, ALU.mult), reads=[ang, cf], writes=[ang])
                        for tab, shift in ((St, 0.0), (Ct, PI / 2)):
                            if shift != 0.0:
                                k.op("dve", lambda e, ang=ang, shift=shift: e.tensor_scalar(ang[:], ang[:], shift, None, ALU.add), reads=[ang], writes=[ang])
                            k.op("dve", lambda e, ang=ang, yy=yy: e.tensor_scalar(yy[:], ang[:], 1.0 / (2 * PI), None, ALU.mult), reads=[ang], writes=[yy])
                            k.op("dve", lambda e, ni=ni, yy=yy: e.tensor_copy(ni[:], yy[:]), reads=[yy], writes=[ni])
                            k.op("dve", lambda e, ni=ni, yy=yy: e.tensor_copy(yy[:], ni[:]), reads=[ni], writes=[yy])
                            k.op("dve", lambda e, ang=ang, yy=yy: e.scalar_tensor_tensor(yy[:], yy[:], -2 * PI, ang[:], ALU.mult, ALU.add), reads=[yy, ang], writes=[yy])
                            k.op("dve", lambda e, yy=yy: e.tensor_scalar(yy[:], yy[:], PI, -PI, ALU.min, ALU.max), reads=[yy], writes=[yy])
                            k.op("act", lambda e, tab=tab, yy=yy, sl=sl: e.activation(tab[:, sl], yy[:], AF.Sin), reads=[yy], writes=[tab])
                    k.barrier()
                k.es = ph
                accA = k.sb("accA", [128, S], F32)
                accB = k.sb("accB", [128, S], F32)
                Wq = [k.sb("Wq", [128, 8, 3, 128], BF16) for _ in range(2)]
                QK = [k.sb("QT", [128, S], BF16), k.sb("KT", [128, S], BF16)]
                Vt2 = k.sb("Vt2", [128, NT, 2, 128], BF16)
                k.op("dve", lambda e: e.memset(Vt2[:, :, :, 64:128], 1.0), writes=[Vt2.r(t_) for t_ in range(NT)])
                VT = k.sb("VT", [128, S], BF16)
                qs = [k.sb("qs", [128, 512], BF16) for _ in range(2)]
                sq = [k.sb("sq", [128, 512], BF16) for _ in range(2)]
                lnv = [k.sb("lnv", [128, 512], F32) for _ in range(2)]
                rstd = [k.sb("rstd", [128, 512], F32) for _ in range(2)]
                ta = [k.sb("ta", [128, 512], F32) for _ in range(2)]
                tb = [k.sb("tb", [128, 512], F32) for _ in range(2)]
                PT = [k.sb("PT", [128, 2, 128], BF16) for _ in range(4)]
                ost = [k.sb("ost", [128, 512], BF16) for _ in range(2)]
                rdn = [k.sb("rdn", [128, 512], F32) for _ in range(2)]
                pq = [k.ps("pq", [128, 512]) for _ in range(2)]
                pss = k.ps("pss", [128, 512])
                prq = k.ps("prq", [128, 512])
                pxa = k.ps("pxa", [128, 512])
                pxb = k.ps("pxb", [128, 512])
                pvt2 = [k.ps("pvt", [128, 128], BF16) for _ in range(2)]
                a_w4 = a_w_in.ap().rearrange("(c p) (gs h e) -> p c gs (h e)", p=128, gs=9, h=16)
                wi = 0
                qi = 0
                pti = 0
                for hp in range(8):
                    for g, d in enumerate((1, 4, 16)):
                        nbk = S // (128 * d)
                        W = Wq[wi % 2]
                        wi += 1
                        for c_ in range(8):
                            k.dma("pool", lambda e, W=W, g=g, hp=hp, c_=c_: e.dma_start(out=W[:, c_, :, :], in_=a_w4[:, c_, 3 * g:3 * g + 3, hp * 128:(hp + 1) * 128]), writes=[W.r(c_)])
                        Wres = [W.r(c_) for c_ in range(8)]

                        def tokv(ap2, r, n0, cnt, d=d):
                            return ap2.rearrange("p (u d) -> p u d", d=d)[:, n0 * 128:n0 * 128 + cnt, r]
                        def qk_a(tc, s_, bufi):
                            sl = slice(tc * 512, (tc + 1) * 512)
                            P_, QS, SQ = pq[bufi % 2], qs[bufi % 2], sq[bufi % 2]
                            for c_ in range(8):
                                k.op("pe", lambda e, c_=c_: e.matmul(P_[:], lhsT=W[:, c_, s_, :], rhs=hT[:, c_, sl], start=(c_ == 0), stop=(c_ == 7)),
                                     reads=Wres + hTres[tc * 4:(tc + 1) * 4], writes=[P_])
                            k.op("act", lambda e: e.activation(QS[:], P_[:], AF.Identity, scale=gcol[:, g, s_:s_ + 1]), reads=[P_, gcol], writes=[QS])
                            k.op("act", lambda e: e.activation(SQ[:], P_[:], AF.Square), reads=[P_], writes=[SQ])

                        def qk_b(tc, s_, bufi):
                            sl = slice(tc * 512, (tc + 1) * 512)
                            QS, SQ, LN, RS, TA, TB = qs[bufi % 2], sq[bufi % 2], lnv[bufi % 2], rstd[bufi % 2], ta[bufi % 2], tb[bufi % 2]
                            k.op("pe", lambda e: e.matmul(pss[:], lhsT=blk, rhs=SQ[:], start=True, stop=True), reads=[SQ, cb], writes=[pss])
                            k.op("pe", lambda e: e.matmul(prq[:], lhsT=RT, rhs=QS[:], start=True, stop=True), reads=[QS, cb], writes=[prq])
                            k.op("act", lambda e: e.activation(LN[:], pss[:], AF.Ln, bias=epsc[:, 0:1], scale=1.0), reads=[pss, epsc], writes=[LN])
                            k.op("act", lambda e: e.activation(RS[:], LN[:], AF.Exp, scale=-0.5), reads=[LN], writes=[RS])
                            k.op("dve", lambda e: e.tensor_tensor(TA[:], QS[:], Ct[:, sl], ALU.mult), reads=[QS, Ct], writes=[TA])
                            k.op("dve", lambda e: e.tensor_tensor(TB[:], prq[:], St[:, sl], ALU.mult), reads=[prq, St], writes=[TB])
                            k.op("dve", lambda e: e.tensor_tensor(TA[:], TA[:], TB[:], ALU.add), reads=[TA, TB], writes=[TA])
                            k.op("dve", lambda e: e.tensor_tensor(QK[s_][:, sl], TA[:], RS[:], ALU.mult), reads=[TA, RS], writes=[QK[s_].r(tc)])
                        qk_items = [(tc, s_) for tc in range(8) for s_ in range(2)]
                        for ii in range(len(qk_items) + 1):
                            if ii < len(qk_items):
                                qk_a(qk_items[ii][0], qk_items[ii][1], qi + ii)
                            if ii >= 1:
                                qk_b(qk_items[ii - 1][0], qk_items[ii - 1][1], qi + ii - 1)
                        qi += len(qk_items)
                        QKres = [[QK[s_].r(tc) for tc in range(8)] for s_ in range(2)]
                        for tc in range(8):
                            sl = slice(tc * 512, (tc + 1) * 512)
                            P_ = pq[qi % 2]
                            qi += 1
                            for c_ in range(8):
                                k.op("pe", lambda e, P_=P_, c_=c_, sl=sl: e.matmul(P_[:], lhsT=W[:, c_, 2, :], rhs=hT[:, c_, sl], start=(c_ == 0), stop=(c_ == 7)),
                                     reads=Wres + hTres[tc * 4:(tc + 1) * 4], writes=[P_])
                            if tc % 2 == 0:
                                k.op("act", lambda e, P_=P_, sl=sl: e.activation(VT[:, sl], P_[:], AF.Identity), reads=[P_], writes=[VT.r(tc)])
                            else:
                                k.op("dve", lambda e, P_=P_, sl=sl: e.tensor_copy(VT[:, sl], P_[:]), reads=[P_], writes=[VT.r(tc)])
                        VTres = [VT.r(tc) for tc in range(8)]
                        for ti in range(NT):
                            PV_ = pvt2[ti % 2]
                            r, n_ = ti // nbk, ti % nbk
                            k.op("pe", lambda e, r=r, n_=n_, PV_=PV_: e.transpose(PV_[:], tokv(VT[:], r, n_, 128), ident_b), reads=VTres + [cb], writes=[PV_])
                            if ti % 2 == 0:
                                k.op("act", lambda e, ti=ti, PV_=PV_: e.activation(Vt2[:, ti, :, 0:64], PV_[:].rearrange("p (h e) -> p h e", h=2), AF.Identity), reads=[PV_], writes=[Vt2.r(ti)])
                            else:
                                k.op("dve", lambda e, ti=ti, PV_=PV_: e.tensor_copy(Vt2[:, ti, :, 0:64], PV_[:].rearrange("p (h e) -> p h e", h=2)), reads=[PV_], writes=[Vt2.r(ti)])
                        items = []
                        for r in range(d):
                            for n0 in range(0, nbk, 4):
                                nblk = min(4, nbk - n0)
                                for h_ in range(2):
                                    for bi in range(nblk):
                                        items.append((r, n0, nblk, h_, bi, h_ == 1 and bi == nblk - 1))

                        def att_a(it, bufi):
                            r, n0, nblk, h_, bi, last = it
                            hs = slice(h_ * 64, (h_ + 1) * 64)
                            n_ = n0 + bi
                            PSb = (pq[0], pq[1], pss, prq)[bufi % 4]
                            psv = PSb[:, 0:256].rearrange("p (a b) -> p a b", a=2)
                            P_T = PT[bufi % 4]
                            qv = tokv(QK[0][hs, :], r, n_, 128)
                            k.op("pe", lambda e: e.matmul(psv[:, 1, :], lhsT=tokv(QK[1][hs, :], r, n_, 128), rhs=qv, start=True, stop=True),
                                 reads=QKres[0] + QKres[1], writes=[PSb])
                            if n_ > 0:
                                k.op("pe", lambda e: e.matmul(psv[:, 0, :], lhsT=tokv(QK[1][hs, :], r, n_ - 1, 128), rhs=qv, start=True, stop=True),
                                     reads=QKres[0] + QKres[1], writes=[PSb])
                                k.op("act", lambda e: e.activation(P_T[:], psv, AF.Exp, scale=0.125), reads=[PSb], writes=[P_T])
                                k.op("pool", lambda e: e.tensor_tensor(P_T[:], P_T[:], mask2, ALU.mult), reads=[P_T, cb2], writes=[P_T])
                            else:
                                k.op("act", lambda e: e.activation(P_T[:, 1, :], psv[:, 1, :], AF.Exp, scale=0.125), reads=[PSb], writes=[P_T])
                                k.op("pool", lambda e: e.tensor_tensor(P_T[:, 1, :], P_T[:, 1, :], mask2[:, 1, :], ALU.mult), reads=[P_T, cb2], writes=[P_T])

                        def att_b(it, bufi):
                            r, n0, nblk, h_, bi, last = it
                            n_ = n0 + bi
                            ti = r * nbk + n_
                            P_T = PT[bufi % 4]
                            PX = pxa if h_ == 0 else pxb
                            cs = slice(bi * 128, (bi + 1) * 128)
                            k.op("pe", lambda e: e.matmul(PX[:, cs], lhsT=Vt2[:, ti, h_, :], rhs=P_T[:, 1, :], start=True, stop=(n_ == 0)),
                                 reads=[P_T, Vt2.r(ti)], writes=[PX])
                            if n_ > 0:
                                k.op("pe", lambda e: e.matmul(PX[:, cs], lhsT=Vt2[:, ti - 1, h_, :], rhs=P_T[:, 0, :], start=False, stop=True),
                                     reads=[P_T, Vt2.r(ti - 1)], writes=[PX])
                            if last:
                                cw = nblk * 128
                                for (PX_, ACC) in ((pxa, accA), (pxb, accB)):
                                    av = tokv(ACC[:], r, n0, cw)
                                    if g == 0:
                                        if PX_ is pxa:
                                            k.op("act", lambda e, av=av, PX_=PX_: e.activation(av, PX_[:, 0:cw], AF.Identity), reads=[PX_], writes=[ACC])
                                        else:
                                            k.op("dve", lambda e, av=av, PX_=PX_: e.tensor_copy(av, PX_[:, 0:cw]), reads=[PX_], writes=[ACC])
                                    else:
                                        k.op("dve", lambda e, av=av, PX_=PX_: e.tensor_tensor(av, PX_[:, 0:cw], av, ALU.add), reads=[PX_, ACC], writes=[ACC])
                        LA = 3
                        for ii in range(len(items) + LA):
                            if ii < len(items):
                                att_a(items[ii], pti + ii)
                            if ii >= LA:
                                att_b(items[ii - LA], pti + ii - LA)
                        pti += len(items)
                    for hx, ACC in enumerate((accA, accB)):
                        for c8 in range(8):
                            sl = slice(c8 * 512, (c8 + 1) * 512)
                            RD, OS = rdn[c8 % 2], ost[c8 % 2]
                            PD_ = pss if c8 % 2 == 0 else prq
                            k.op("pe", lambda e, PD_=PD_, ACC=ACC, sl=sl: e.matmul(PD_[0:64, :], lhsT=ident_f[:, 64:128], rhs=ACC[:, sl], start=True, stop=True), reads=[ACC, cf], writes=[PD_])
                            k.op("act", lambda e, RD=RD, PD_=PD_: e.activation(RD[0:64, :], PD_[0:64, :], AF.Ln), reads=[PD_], writes=[RD])
                            k.op("act", lambda e, RD=RD: e.activation(RD[0:64, :], RD[0:64, :], AF.Exp, scale=-1.0), reads=[RD], writes=[RD])
                            k.op("dve", lambda e, RD=RD, OS=OS, ACC=ACC, sl=sl: e.tensor_tensor(OS[0:64, :], ACC[0:64, sl], RD[0:64, :], ALU.mult), reads=[RD, ACC], writes=[OS])
                            k.dma("sp", lambda e, OS=OS, hp=hp, hx=hx, sl=sl: e.dma_start(out=OT[hp * 128 + hx * 64:hp * 128 + (hx + 1) * 64, sl], in_=OS[0:64, :]), reads=[OS], writes=[OT.r((hp, hx, c8))])
                k.barrier()
            k.es = root
            out_proj(l, a_w_out, xsrc, xdst)

        def gla_layer(l, xsrc, xdst):
            with ExitStack() as ph:
                k.es = ph
                hT = k.sb("hT", [128, 8, S], BF16)
                build_hT(l, xsrc, hT)
                k.es = ph
                hTres = [hT.r(t) for t in range(NT)]
                Wb = k.sb("Wb", [128, 8, 3088], BF16)
                bw3 = b_w_in.ap().rearrange("(c p) n -> p c n", p=128)
                for c_ in range(8):
                    for hf in range(2):
                        k.dma("pool", lambda e, c_=c_, hf=hf: e.dma_start(out=Wb[:, c_, hf * 1544:(hf + 1) * 1544], in_=bw3[:, c_, hf * 1544:(hf + 1) * 1544]), writes=[Wb.r((c_, hf))])
                Wbres = [Wb.r((c_, hf)) for c_ in range(8) for hf in range(2)]
                Wg = k.sb("Wg", [16, 512], BF16)
                k.dma("pool", lambda e: e.dma_start(out=Wg[:], in_=b_w_gate_up.ap()), writes=[Wg])
                negb = k.sb("negb", [128, 4], F32)
                with nc.allow_non_contiguous_dma(reason="tiny bias load"):
                    k.dma("sp", lambda e: e.dma_start(out=negb[:], in_=b_gate_bias.ap().rearrange("o (h p) -> p (o h)", p=128)), writes=[negb])
                k.op("dve", lambda e: e.tensor_scalar(negb[:], negb[:], -1.0, None, ALU.mult), reads=[negb], writes=[negb])
                onec = k.sb("onec", [128, 1], F32)
                k.op("dve", lambda e: e.memset(onec[:], 1.0), writes=[onec])
                epsc = k.sb("epsc", [128, 1], F32)
                k.op("dve", lambda e: e.memset(epsc[:], EPS), writes=[epsc])
                ogain = k.sb("ogain", [128, 256], F32)
                k.dma("sp", lambda e: e.dma_start(out=ogain[:], in_=b_out_gain.ap().partition_broadcast(128)), writes=[ogain])
                rmask = k.sb("rmask", [128, 512], F32)
                k.op("dve", lambda e: e.memset(rmask[:], 1.0), writes=[rmask])
                k.op("dve", lambda e: e.memset(rmask[:].rearrange("p (a b) -> p a b", b=64)[:, :, 0:1], 0.0), reads=[rmask], writes=[rmask])
                S32 = [k.sb("S32", [128, 256], F32) for _ in range(4)]
                for h_ in range(4):
                    k.op("dve", lambda e, h_=h_: e.memset(S32[h_][:], 0.0), writes=[S32[h_]])
                aT = k.sb("aT", [16, 512], BF16)
                e1 = [k.sb("e1", [128, 512], F32) for _ in range(2)]
                cs = [k.sb("cs", [128, 512], F32) for _ in range(2)]
                Ep = [k.sb("Ep", [128, 512], F32) for _ in range(2)]
                En = [k.sb("En", [128, 512], F32) for _ in range(2)]
                decs = k.sb("decs", [128, 4, 8], F32)
                QD = k.sb("QD", [128, 4, 512], BF16)
                KN = k.sb("KN", [128, 4, 512], BF16)
                KD = k.sb("KD", [128, 4, 512], BF16)
                QD32 = k.sb("QD32", [128, 4, 512], F32)
                vb = [k.sb("vb", [128, D], BF16) for _ in range(2)]
                sr = [k.sb("sr", [128, D], F32) for _ in range(2)]
                knT = [k.sb("knT", [128, 128], BF16) for _ in range(2)]
                Pm = [k.sb("Pm", [128, 128], BF16) for _ in range(2)]
                st2 = [k.sb("st2", [128, 4, 2], F32) for _ in range(2)]
                junk = k.sb("gjunk", [128, 256], BF16)
                onrm = [k.sb("onrm", [128, 256], F32) for _ in range(2)]
                ofin = [k.sb("ofin", [128, D], BF16) for _ in range(2)]
                oTs = [k.sb("oTs", [128, 8, 128], BF16) for _ in range(2)]
                pA = [k.ps("pA", [128, 512]) for _ in range(2)]
                pCs = [k.ps("pC", [128, 256]) for _ in range(2)]
                pkvs = [k.ps("pkv", [128, 256]) for _ in range(2)]
                patt = k.ps("patt", [128, 128])
                pOT = k.ps("pOT", [128, 8, 128], BF16)
                ai = 0
                ci = 0
                OTv = OT[:, :].rearrange("(c p) s -> p c s", p=128)
                vr_done = set()
                aic = [0]

                def proj_vr(t):
                    vr_done.add(t)
                    tl = slice(t * 128, (t + 1) * 128)
                    VB, SR = vb[t % 2], sr[t % 2]
                    for nh in range(2):
                        PA = pA[aic[0] % 2]
                        aic[0] += 1
                        for c_ in range(8):
                            k.op("pe", lambda e, c_=c_: e.matmul(PA[:], lhsT=hT[:, c_, tl], rhs=Wb[:, c_, 1024 + nh * 512:1024 + (nh + 1) * 512], start=(c_ == 0), stop=(c_ == 7)), reads=Wbres + [hT.r(t)], writes=[PA])
                        k.op("dve", lambda e: e.tensor_copy(VB[:, nh * 512:(nh + 1) * 512], PA[:]), reads=[PA], writes=[VB.r(nh)])
                    for nh in range(2):
                        PA = pA[aic[0] % 2]
                        aic[0] += 1
                        for c_ in range(8):
                            k.op("pe", lambda e, c_=c_: e.matmul(PA[:], lhsT=hT[:, c_, tl], rhs=Wb[:, c_, 2048 + nh * 512:2048 + (nh + 1) * 512], start=(c_ == 0), stop=(c_ == 7)), reads=Wbres + [hT.r(t)], writes=[PA])
                        k.op("act", lambda e: e.activation(SR[:, nh * 512:(nh + 1) * 512], PA[:], AF.Silu), reads=[PA], writes=[SR.r(nh)])
                for mc in range(8):
                    sl = slice(mc * 512, (mc + 1) * 512)
                    hr = hTres[mc * 4:(mc + 1) * 4]
                    PA = pA[ai % 2]
                    ai += 1
                    for c_ in range(8):
                        k.op("pe", lambda e, PA=PA, c_=c_, sl=sl: e.matmul(PA[0:16, :], lhsT=Wb[:, c_, 3072:3088], rhs=hT[:, c_, sl], start=(c_ == 0), stop=(c_ == 7)), reads=Wbres + hr, writes=[PA])
                    k.op("act", lambda e, PA=PA: e.activation(aT[:], PA[0:16, :], AF.Identity), reads=[PA], writes=[aT])
                    for h_ in range(4):
                        E1, CS, EP, EN = e1[h_ % 2], cs[h_ % 2], Ep[h_ % 2], En[h_ % 2]
                        PA = pA[ai % 2]
                        ai += 1
                        k.op("pe", lambda e, PA=PA, h_=h_: e.matmul(PA[:], lhsT=Wg[0:16, h_ * 128:(h_ + 1) * 128], rhs=aT[0:16, :], start=True, stop=True), reads=[Wg, aT], writes=[PA])
                        k.op("act", lambda e, PA=PA, E1=E1, h_=h_: e.activation(E1[:], PA[:], AF.Exp, bias=negb[:, h_:h_ + 1], scale=-1.0), reads=[PA, negb], writes=[E1])
                        k.op("act", lambda e, E1=E1: e.activation(E1[:], E1[:], AF.Ln, bias=onec[:, 0:1], scale=1.0), reads=[E1, onec], writes=[E1])
                        k.op("dve", lambda e, E1=E1, CS=CS: e.tensor_tensor_scan(CS[:], rmask[:], E1[:], 0.0, ALU.mult, ALU.add), reads=[E1, rmask], writes=[CS])
                        k.op("act", lambda e, CS=CS, EP=EP: e.activation(EP[:], CS[:], AF.Exp, scale=-1.0 / 16), reads=[CS], writes=[EP])
                        k.op("act", lambda e, CS=CS, EN=EN: e.activation(EN[:], CS[:], AF.Exp, scale=1.0 / 16), reads=[CS], writes=[EN])
                        k.op("dve", lambda e, EP=EP, h_=h_: e.tensor_copy(decs[:, h_, :], EP[:].rearrange("p (a b) -> p a b", b=64)[:, :, 63]), reads=[EP], writes=[decs.r(h_)])
                        PA = pA[ai % 2]
                        ai += 1
                        for c_ in range(8):
                            k.op("pe", lambda e, PA=PA, c_=c_, sl=sl, h_=h_: e.matmul(PA[:], lhsT=Wb[:, c_, h_ * 128:(h_ + 1) * 128], rhs=hT[:, c_, sl], start=(c_ == 0), stop=(c_ == 7)), reads=Wbres + hr, writes=[PA])
                        k.op("dve", lambda e, PA=PA, EP=EP, h_=h_: e.scalar_tensor_tensor(QD[:, h_, :], PA[:], float(128 ** -0.5), EP[:], ALU.mult, ALU.mult), reads=[PA, EP], writes=[QD.r(h_)])
                        k.op("dve", lambda e, PA=PA, EP=EP, h_=h_: e.scalar_tensor_tensor(QD32[:, h_, :], PA[:], float(128 ** -0.5), EP[:], ALU.mult, ALU.mult), reads=[PA, EP], writes=[QD32.r(h_)])
                        PA = pA[ai % 2]
                        ai += 1
                        for c_ in range(8):
                            k.op("pe", lambda e, PA=PA, c_=c_, sl=sl, h_=h_: e.matmul(PA[:], lhsT=Wb[:, c_, 512 + h_ * 128:512 + (h_ + 1) * 128], rhs=hT[:, c_, sl], start=(c_ == 0), stop=(c_ == 7)), reads=Wbres + hr, writes=[PA])
                        k.op("dve", lambda e, PA=PA, EN=EN, h_=h_: e.tensor_tensor(KN[:, h_, :], PA[:], EN[:], ALU.mult), reads=[PA, EN], writes=[KN.r(h_)])
                        k.op("dve", lambda e, h_=h_: e.tensor_tensor(KD[:, h_, :].rearrange("p (a b) -> p a b", b=64), KN[:, h_, :].rearrange("p (a b) -> p a b", b=64),
                                                               decs[:, h_, :].unsqueeze(2).to_broadcast([128, 8, 64]), ALU.mult), reads=[KN.r(h_), decs.r(h_)], writes=[KD.r(h_)])
                    for t4 in range(4):
                        t = mc * 4 + t4
                        tl = slice(t * 128, (t + 1) * 128)
                        ml = slice(t4 * 128, (t4 + 1) * 128)
                        VB, SR, OF, OTS = vb[t % 2], sr[t % 2], ofin[t % 2], oTs[t % 2]
                        if t not in vr_done:
                            proj_vr(t)
                        for pair in range(2):
                            hh = (2 * pair, 2 * pair + 1)
                            for h_ in hh:
                                hb = h_ % 2
                                KT_, PM = knT[hb], Pm[hb]
                                vh = slice(h_ * 256, (h_ + 1) * 256)
                                vres = VB.r(h_ // 2)
                                k.op("pe", lambda e, h_=h_: e.transpose(pOT[:, 0, :], KD[:, h_, ml], ident_b), reads=[KD.r(h_), cb], writes=[pOT])
                                k.op("act", lambda e, KT_=KT_: e.activation(KT_[:], pOT[:, 0, :], AF.Identity), reads=[pOT], writes=[KT_])
                                k.op("pe", lambda e, h_=h_: e.matmul(patt[:], lhsT=KN[:, h_, ml], rhs=QD[:, h_, ml], start=True, stop=True), reads=[KN.r(h_), QD.r(h_)], writes=[patt])
                                k.op("dve", lambda e, PM=PM: e.tensor_tensor(PM[:], patt[:], gmask, ALU.mult), reads=[patt, cb2], writes=[PM])
                                k.op("pe", lambda e, PM=PM, vh=vh, hb=hb: e.matmul(pCs[hb][:], lhsT=PM[:], rhs=VB[:, vh], start=True, stop=False), reads=[PM, vres], writes=[pCs[hb]])
                            if pair == 0 and t + 1 < NT:
                                proj_vr(t + 1)
                            for half in range(2):
                                ps_ = slice(half * 64, (half + 1) * 64)
                                mq = slice(t4 * 128 + half * 64, t4 * 128 + (half + 1) * 64)
                                cidx = t4 * 2 + half
                                for h_ in hh:
                                    hb = h_ % 2
                                    KT_ = knT[hb]
                                    vh = slice(h_ * 256, (h_ + 1) * 256)
                                    vres = VB.r(h_ // 2)
                                    k.op("pe", lambda e, h_=h_, hb=hb: e.matmul(pCs[hb][ps_, :], lhsT=QD32[:, h_, mq], rhs=S32[h_][:], start=False, stop=(half == 1)),
                                         reads=[QD32.r(h_), S32[h_]], writes=[pCs[hb]])
                                    k.op("pe", lambda e, KT_=KT_, vh=vh, hb=hb: e.matmul(pkvs[hb][:], lhsT=KT_[ps_, :], rhs=VB[ps_, vh], start=True, stop=True), reads=[KT_, vres], writes=[pkvs[hb]])
                                for h_ in hh:
                                    hb = h_ % 2
                                    k.op("dve", lambda e, h_=h_, hb=hb: e.scalar_tensor_tensor(S32[h_][:], S32[h_][:], decs[:, h_, cidx:cidx + 1], pkvs[hb][:], ALU.mult, ALU.add),
                                         reads=[pkvs[hb], S32[h_], decs.r(h_)], writes=[S32[h_]])
                            ST2 = st2[pair]
                            for h_ in hh:
                                hb = h_ % 2
                                k.op("act", lambda e, hb=hb: e.activation(junk[:], pCs[hb][:], AF.Square, accum_out=ST2[:, 0, hb:hb + 1]), reads=[pCs[hb]], writes=[junk, ST2])
                            k.op("dve", lambda e: e.tensor_scalar(ST2[:, 1, :], ST2[:, 0, :], 1.0 / 256, EPS, ALU.mult, ALU.add), reads=[ST2], writes=[ST2])
                            k.op("act", lambda e: e.activation(ST2[:, 2, :], ST2[:, 1, :], AF.Ln), reads=[ST2], writes=[ST2])
                            k.op("act", lambda e: e.activation(ST2[:, 3, :], ST2[:, 2, :], AF.Exp, scale=-0.5), reads=[ST2], writes=[ST2])
                            for h_ in hh:
                                hb = h_ % 2
                                ON = onrm[hb]
                                vh = slice(h_ * 256, (h_ + 1) * 256)
                                k.op("dve", lambda e, hb=hb, ON=ON: e.scalar_tensor_tensor(ON[:], pCs[hb][:], ST2[:, 3, hb:hb + 1], ogain[:], ALU.mult, ALU.mult), reads=[pCs[hb], ST2, ogain], writes=[ON])
                                k.op("dve", lambda e, ON=ON, vh=vh, h_=h_: e.tensor_tensor(OF[:, vh], ON[:], SR[:, vh], ALU.mult), reads=[ON, SR.r(h_ // 2)], writes=[OF.r(h_)])
                        for c_ in range(8):
                            k.op("pe", lambda e, c_=c_, OF=OF: e.transpose(pOT[:, c_, :], OF[:, c_ * 128:(c_ + 1) * 128], ident_b), reads=[OF.r(c_ // 2), cb], writes=[pOT])
                        k.op("act", lambda e, OTS=OTS: e.activation(OTS[:], pOT[:], AF.Identity), reads=[pOT], writes=[OTS])
                        k.dma("sp", lambda e, OTS=OTS, tl=tl: e.dma_start(out=OTv[:, :, tl], in_=OTS[:]), reads=[OTS], writes=[OT.r(("g", t))])
                k.barrier()
            k.es = root
            out_proj(l, b_w_out, xsrc, xdst)

        XIN = Buf(x_in, "x")
        attn_layer(0, XIN, xs1)
        k.es = root
        moe_layer(0, xs1, xs2)
        k.es = root
        gla_layer(1, xs2, xs1)
        k.es = root
        moe_layer(1, xs1, outb)
        k.es = root
        k.barrier()
        print("instructions:", k.ninst)
    return nc


def make_consts():
    c = np.zeros((128, 1024), np.float32)
    p = np.arange(128)
    c[:, 0:128] = np.eye(128)
    c[:, 128:256] = (p[:, None] < p[None, :]).astype(np.float32)
    c[:, 256:288] = np.arange(32)[None, :]
    c[:, 288:296] = (np.arange(8) * SB)[None, :]
    c[:, 296:360] = np.arange(64)[None, :]
    c[:, 360:368] = np.arange(8)[None, :] * 128 + p[:, None]
    c[:, 368] = p
    half = 32
    inv = (10000.0 ** (-np.arange(half, dtype=np.float32) / half)).astype(np.float32)
    c[:, 369] = inv[p % 32]
    RT = np.zeros((128, 128), np.float32)
    for hh in range(2):
        for e in range(64):
            if e < 32:
                RT[hh * 64 + e + 32, hh * 64 + e] = -1.0
            else:
                RT[hh * 64 + e - 32, hh * 64 + e] = 1.0
    c[:, 384:512] = RT
    blk = np.zeros((128, 128), np.float32)
    blk[:64, :64] = 1.0 / 64
    blk[64:, 64:] = 1.0 / 64
    c[:, 512:640] = blk
    c[:, 640:768] = np.where(p[:, None] <= p[None, :], 0.0, NEGM)
    c[:, 768:896] = np.where(p[:, None] >= p[None, :], 0.0, NEGM)
    c[:, 896:1024] = np.eye(128)
    return c


def make_consts2():
    p = np.arange(128)
    c = np.zeros((128, 384), np.float32)
    c[:, 0:128] = ((p[:, None] // 64 == p[None, :] // 64) & (p[:, None] <= p[None, :]))
    c[:, 128:256] = (p[:, None] >= p[None, :])
    c[:, 256:384] = (p[:, None] <= p[None, :])
    return c


_NC = None
_DEBUG = False


def kernel(**inp):
    global _NC
    if _NC is None:
        _NC = build_program(debug=_DEBUG)
    cst = make_consts()
    shared = {
        "ada_w": inp["ada_w"], "ada_b": inp["ada_b"], "norm1_g": inp["norm1_g"], "norm2_g": inp["norm2_g"],
        "a_w_in": inp["a_w_in"][0], "a_q_gain": inp["a_q_gain"][0], "a_k_gain": inp["a_k_gain"][0],
        "a_w_out": inp["a_w_out"][0], "b_w_in": inp["b_w_in"][0], "b_w_gate_up": inp["b_w_gate_up"][0],
        "b_gate_bias": inp["b_gate_bias"], "b_out_gain": inp["b_out_gain"], "b_w_out": inp["b_w_out"][0],
        "router_w": inp["router_w"], "router_b": inp["router_b"],
        "moe_w_gu": inp["moe_w_gu"].reshape(2, NE * D, 2 * D), "moe_b_gu": inp["moe_b_gu"],
        "moe_w_down": inp["moe_w_down"].reshape(2, NE * D, D), "moe_b_down": inp["moe_b_down"],
        "cst": cst, "cst2": make_consts2(),
    }
    shared = {kk: np.ascontiguousarray(v, dtype=np.float32) for kk, v in shared.items()}
    in_maps = []
    for b in range(8):
        m = dict(shared)
        m["x"] = np.ascontiguousarray(inp["x"][b])
        m["c"] = np.ascontiguousarray(inp["c"][b:b + 1])
        m["pos"] = np.ascontiguousarray(inp["positions"][b:b + 1]).astype(np.int32)
        in_maps.append(m)
    if _DEBUG:
        return run_bass_kernel_spmd(_NC, in_maps, core_ids=list(range(8)))
    res = run_bass_kernel_spmd(_NC, in_maps, core_ids=list(range(8)))
    return np.stack([res.results[b]["out"] for b in range(8)], axis=0)
```

```python
import numpy as np
from contextlib import ExitStack
import concourse.bass as bass
import concourse.mybir as mybir
from concourse.bass_utils import run_bass_kernel_spmd

F32 = mybir.dt.float32
BF16 = mybir.dt.bfloat16
I32 = mybir.dt.int32
U32 = mybir.dt.uint32
AF = mybir.ActivationFunctionType
ALU = mybir.AluOpType
AX = mybir.AxisListType

D = 1024
S = 4096
NT = S // 128
NE = 32
SB = 512
NSB = 63
NSLOT = NSB * SB
EPS = 1e-6
ALPHA = 1.702
EPOCH = 20000
NEGM = -30000.0
DBG_LAYER = 1


class Res:
    __slots__ = ("name", "lw", "rd")

    def __init__(self, name=""):
        self.name = name
        self.lw = None
        self.rd = []


class Buf:
    def __init__(self, t, name):
        self.t = t
        self.name = name
        self.res = {}

    def r(self, key=0):
        x = self.res.get(key)
        if x is None:
            x = Res(f"{self.name}:{key}")
            self.res[key] = x
        return x

    def __getitem__(self, idx):
        return self.t[idx]


class K:
    def __init__(self, nc, es):
        self.nc = nc
        self.es = es
        self.eng = {"pe": nc.tensor, "dve": nc.vector, "act": nc.scalar,
                    "pool": nc.gpsimd, "sp": nc.sync}
        self.esem, self.ecnt, self.eepoch = {}, {}, {}
        self.allsems = []
        for e in self.eng:
            self.eepoch[e] = 0
            self.esem[e] = self._newsem(f"p_{e}_0")
            self.ecnt[e] = 0
        self.seen = {e: {} for e in self.eng}
        self.dsem, self.dcnt, self.dnext = {}, {}, {}
        for q in ("sp", "pool"):
            n = 16
            self.dsem[q] = [self._newsem(f"d_{q}_{i}") for i in range(n)]
            self.dcnt[q] = [0] * n
            self.dnext[q] = 0
        self.nbuf = 0
        self.ninst = 0
        self.noself = {"pe"}
        self.last = {}

    def _newsem(self, name):
        s = self.es_root().enter_context(self.nc.semaphore(name))
        self.allsems.append(s)
        return s

    def es_root(self):
        return self._root if hasattr(self, "_root") else self.es

    def sb(self, name, shape, dt):
        self.nbuf += 1
        t = self.es.enter_context(self.nc.sbuf_tensor(f"{name}_{self.nbuf}", shape, dt))
        return Buf(t, name)

    def ps(self, name, shape, dt=F32):
        self.nbuf += 1
        t = self.es.enter_context(self.nc.psum_tensor(f"{name}_{self.nbuf}", shape, dt))
        return Buf(t, name)

    def _wait(self, e, sem, val):
        s = self.seen[e]
        key = id(sem)
        if s.get(key, 0) < val:
            self.eng[e].wait_ge(sem, val)
            s[key] = val

    def _deps(self, e, reads, writes):
        need = {}

        def add(tok):
            if tok is None:
                return
            sem, val = tok
            kk = id(sem)
            if kk not in need or need[kk][1] < val:
                need[kk] = (sem, val)
        for r in reads:
            add(r.lw)
        for w in writes:
            add(w.lw)
            for t in w.rd:
                add(t)
        own = id(self.esem[e])
        for sem, val in need.values():
            if e in self.noself and id(sem) == own:
                continue
            self._wait(e, sem, val)

    def _commit(self, tok, reads, writes):
        for r in reads:
            r.rd.append(tok)
            if len(r.rd) > 48:
                best = {}
                for s, v in r.rd:
                    if id(s) not in best or best[id(s)][1] < v:
                        best[id(s)] = (s, v)
                r.rd = list(best.values())
        for w in writes:
            w.lw = tok
            w.rd = []

    @staticmethod
    def _norm(lst):
        return [x.r() if isinstance(x, Buf) else x for x in lst]

    def op(self, e, fn, reads=(), writes=()):
        reads, writes = self._norm(reads), self._norm(writes)
        if self.ecnt[e] >= EPOCH:
            self.eepoch[e] += 1
            self.esem[e] = self._newsem(f"p_{e}_{self.eepoch[e]}")
            self.ecnt[e] = 0
        self._deps(e, reads, writes)
        inst = fn(self.eng[e])
        self.ecnt[e] += 1
        self.ninst += 1
        inst.then_inc(self.esem[e], 1)
        tok = (self.esem[e], self.ecnt[e])
        self.last[e] = tok
        self._commit(tok, reads, writes)
        return tok

    def dma(self, q, fn, reads=(), writes=()):
        reads, writes = self._norm(reads), self._norm(writes)
        i = self.dnext[q]
        self.dnext[q] = (i + 1) % len(self.dsem[q])
        sem = self.dsem[q][i]
        if self.dcnt[q][i] > 0:
            self._wait(q, sem, 16 * self.dcnt[q][i])
        self._deps(q, reads, writes)
        inst = fn(self.eng[q])
        self.dcnt[q][i] += 1
        self.ninst += 1
        inst.then_inc(sem, 16)
        tok = (sem, 16 * self.dcnt[q][i])
        self._commit(tok, reads, writes)
        return tok

    def barrier(self):
        toks = list(self.last.values())
        for q in self.dsem:
            for i, s in enumerate(self.dsem[q]):
                if self.dcnt[q][i]:
                    toks.append((s, 16 * self.dcnt[q][i]))
        for e in self.eng:
            for sem, val in toks:
                self._wait(e, sem, val)


def build_program(debug=False):
    nc = bass.Bass("TRN2", target_bir_lowering=False)

    def din(name, shape, dt=F32):
        return nc.dram_tensor(name, shape, dt, kind="ExternalInput")

    def dscr(name, shape, dt=F32):
        return Buf(nc.dram_tensor(name, shape, dt, kind="Internal"), name)

    x_in = din("x", [S, D])
    c_in = din("c", [1, D])
    pos_in = din("pos", [1, S], I32)
    ada_w = din("ada_w", [2, D, 6 * D])
    ada_b = din("ada_b", [2, 6 * D])
    norm1_g = din("norm1_g", [2, D])
    norm2_g = din("norm2_g", [2, D])
    a_w_in = din("a_w_in", [D, 9216])
    a_q_gain = din("a_q_gain", [3, 64])
    a_k_gain = din("a_k_gain", [3, 64])
    a_w_out = din("a_w_out", [D, D])
    b_w_in = din("b_w_in", [D, 3088])
    b_w_gate_up = din("b_w_gate_up", [16, 512])
    b_gate_bias = din("b_gate_bias", [1, 512])
    b_out_gain = din("b_out_gain", [1, 256])
    b_w_out = din("b_w_out", [D, D])
    router_w = din("router_w", [2, D, NE])
    router_b = din("router_b", [2, NE])
    moe_w_gu = din("moe_w_gu", [2, NE * D, 2 * D])
    moe_b_gu = din("moe_b_gu", [2, NE, 2 * D])
    moe_w_down = din("moe_w_down", [2, NE * D, D])
    moe_b_down = din("moe_b_down", [2, NE, D])
    cst = din("cst", [128, 1024])
    cst2 = din("cst2", [128, 384])
    out = nc.dram_tensor("out", [S, D], F32, kind="ExternalOutput")
    outb = Buf(out, "out")
    dbg = Buf(nc.dram_tensor("dbg", [S, D], F32, kind="ExternalOutput"), "dbg") if debug else None

    modbc = dscr("modbc", [2, 6, 128, D])
    xs1 = dscr("xs1", [S, D])
    xs2 = dscr("xs2", [S, D])
    Xs = dscr("Xs", [NSLOT, D], BF16)
    Ys = dscr("Ys", [NSLOT, D])
    OT = dscr("OT", [D, S], BF16)

    with ExitStack() as root:
        k = K.__new__(K)
        k._root = root
        K.__init__(k, nc, root)

        cf = k.sb("cf", [128, 1024], F32)
        k.dma("sp", lambda e: e.dma_start(out=cf[:], in_=cst.ap()), writes=[cf])
        ident_f = cf[:, 0:128]
        Uex = cf[:, 128:256]
        iota_e = cf[:, 256:288]
        thr8 = cf[:, 288:296]
        jidx = cf[:, 296:296 + NSB]
        pidx = cf[:, 360:368]
        iota_p = cf[:, 368:369]
        invf = cf[:, 369:370]
        cb = k.sb("cb", [128, 640], BF16)
        k.op("dve", lambda e: e.tensor_copy(cb[:], cf[:, 384:1024]), reads=[cf], writes=[cb])
        RT = cb[:, 0:128]
        blk = cb[:, 128:256]
        mcur = cb[:, 256:384]
        mprev = cb[:, 384:512]
        ident_b = cb[:, 512:640]
        cf2 = k.sb("cf2", [128, 384], F32)
        k.dma("sp", lambda e: e.dma_start(out=cf2[:], in_=cst2.ap()), writes=[cf2])
        cb2 = k.sb("cb2", [128, 384], BF16)
        k.op("dve", lambda e: e.tensor_copy(cb2[:], cf2[:]), reads=[cf2], writes=[cb2])
        gmask = cb2[:, 0:128]
        mask2 = cb2[:, 128:384].rearrange("p (a b) -> p a b", a=2)
        ones_f = k.sb("ones_f", [128, 128], F32)
        k.op("dve", lambda e: e.memset(ones_f[:], 1.0), writes=[ones_f])
        ones_b = k.sb("ones_b", [128, 512], BF16)
        k.op("dve", lambda e: e.memset(ones_b[:], 1.0), writes=[ones_b])
        PERS = (k.sb("widx", [128, NSB, 8], I32), k.sb("ejb", [128, NSB], F32),
                k.sb("dkp", [128, NT, 4], I32), k.sb("gkp", [128, NT, 4], F32))

        with ExitStack() as ph:
            k.es = ph
            cc = k.sb("cc", [128, 8], F32)
            with nc.allow_non_contiguous_dma(reason="tiny c load"):
                k.dma("sp", lambda e: e.dma_start(out=cc[:], in_=c_in.ap().rearrange("o (kc p) -> p (o kc)", p=128)), writes=[cc])
            k.op("act", lambda e: e.activation(cc[:], cc[:], AF.Silu), reads=[cc], writes=[cc])
            crep = k.sb("crep", [128, 8, 128], F32)
            k.op("dve", lambda e: e.tensor_copy(crep[:], cc[:].unsqueeze(2).to_broadcast([128, 8, 128])), reads=[cc], writes=[crep])
            wa = [k.sb("wa", [128, 3072], F32) for _ in range(2)]
            brow = k.sb("brow", [1, 6 * D], F32)
            pm = [k.ps("pm", [128, 512]) for _ in range(6)]
            stg = [k.sb("stg", [128, 512], F32) for _ in range(2)]
            it = 0
            for l in range(2):
                k.dma("sp", lambda e, l=l: e.dma_start(out=brow[:], in_=ada_b.ap()[l:l + 1, :]), writes=[brow])
                for half in range(2):
                    for kc in range(8):
                        w = wa[it % 2]
                        it += 1
                        k.dma("sp", lambda e, w=w, l=l, kc=kc, half=half: e.dma_start(
                            out=w[:], in_=ada_w.ap()[l, kc * 128:(kc + 1) * 128, half * 3072:(half + 1) * 3072]), writes=[w])
                        for g in range(6):
                            k.op("pe", lambda e, w=w, g=g, kc=kc: e.matmul(pm[g][:], lhsT=crep[:, kc, :], rhs=w[:, g * 512:(g + 1) * 512],
                                                                           start=(kc == 0), stop=False), reads=[w, crep], writes=[pm[g]])
                    for g in range(6):
                        col = half * 3072 + g * 512
                        k.op("pe", lambda e, g=g, col=col: e.matmul(pm[g][:], lhsT=ones_f[0:1, :], rhs=brow[0:1, col:col + 512],
                                                                    start=False, stop=True), reads=[brow, ones_f], writes=[pm[g]])
                        st = stg[g % 2]
                        k.op("act", lambda e, st=st, g=g: e.activation(st[:], pm[g][:], AF.Identity), reads=[pm[g]], writes=[st])
                        slot, hf = col // 1024, (col % 1024) // 512
                        k.dma("sp", lambda e, st=st, l=l, slot=slot, hf=hf: e.dma_start(
                            out=modbc[l, slot, :, hf * 512:(hf + 1) * 512], in_=st[:]), reads=[st], writes=[modbc.r((l, slot))])
            k.barrier()

        def load_mod(l, which, normg):
            SH = k.sb("SH", [128, D], F32)
            G = k.sb("G", [128, D], F32)
            gt = k.sb("gt", [128, D], F32)
            b = 3 * which
            k.dma("sp", lambda e: e.dma_start(out=SH[:], in_=modbc[l, b + 0]), reads=[modbc.r((l, b))], writes=[SH])
            k.dma("sp", lambda e: e.dma_start(out=G[:], in_=modbc[l, b + 1]), reads=[modbc.r((l, b + 1))], writes=[G])
            k.dma("sp", lambda e: e.dma_start(out=gt[:], in_=normg.ap()[l:l + 1, :].partition_broadcast(128)), writes=[gt])
            k.op("dve", lambda e: e.scalar_tensor_tensor(G[:], G[:], 1.0, gt[:], ALU.add, ALU.mult), reads=[G, gt], writes=[G])
            return G, SH

        def load_gate(l, which):
            GT = k.sb("GT", [128, D], F32)
            k.dma("sp", lambda e: e.dma_start(out=GT[:], in_=modbc[l, 3 * which + 2]), reads=[modbc.r((l, 3 * which + 2))], writes=[GT])
            return GT

        class NormCtx:
            pass

        def make_norm():
            n = NormCtx()
            n.xt = [k.sb("n_xt", [128, D], F32) for _ in range(2)]
            n.junk = k.sb("n_junk", [128, D], BF16)
            n.ss = [k.sb("n_ss", [128, 4], F32) for _ in range(2)]
            n.h = [k.sb("n_h", [128, D], F32) for _ in range(2)]
            n.pt = [k.ps("n_pt", [128, 512]) for _ in range(2)]
            n.i = 0
            return n

        def norm_tile(n, src, srcres, t, G, SH):
            i = n.i
            n.i += 1
            xt, ss, h = n.xt[i % 2], n.ss[i % 2], n.h[i % 2]
            k.dma("sp", lambda e: e.dma_start(out=xt[:], in_=src[t * 128:(t + 1) * 128, :]), reads=srcres, writes=[xt])
            k.op("act", lambda e: e.activation(n.junk[:], xt[:], AF.Square, accum_out=ss[:, 0:1]), reads=[xt], writes=[n.junk, ss])
            k.op("dve", lambda e: e.tensor_scalar(ss[:, 1:2], ss[:, 0:1], 1.0 / D, EPS, ALU.mult, ALU.add), reads=[ss], writes=[ss])
            k.op("act", lambda e: e.activation(ss[:, 2:3], ss[:, 1:2], AF.Ln), reads=[ss], writes=[ss])
            k.op("act", lambda e: e.activation(ss[:, 3:4], ss[:, 2:3], AF.Exp, scale=-0.5), reads=[ss], writes=[ss])
            k.op("dve", lambda e: e.scalar_tensor_tensor(h[:], xt[:], ss[:, 3:4], G[:], ALU.mult, ALU.mult), reads=[xt, ss, G], writes=[h])
            k.op("dve", lambda e: e.tensor_tensor(h[:], h[:], SH[:], ALU.add), reads=[h, SH], writes=[h])
            return xt, h

        def transpose_to(n, h, dsts):
            for half in range(2):
                pt = n.pt[half]
                for j in range(4):
                    kc = half * 4 + j
                    k.op("pe", lambda e, pt=pt, j=j, kc=kc: e.transpose(pt[:, j * 128:(j + 1) * 128], h[:, kc * 128:(kc + 1) * 128], ident_f),
                         reads=[h, cf], writes=[pt])
                for (fn, res, eng) in dsts:
                    o = fn(half)
                    if eng == "act":
                        k.op("act", lambda e, o=o, pt=pt: e.activation(o, pt[:].rearrange("p (a b) -> p a b", a=4), AF.Identity), reads=[pt], writes=res)
                    else:
                        k.op("dve", lambda e, o=o, pt=pt: e.tensor_copy(o, pt[:].rearrange("p (a b) -> p a b", a=4)), reads=[pt], writes=res)

        def moe_layer(l, xsrc, xdst):
            widx, ejb, dkp, gkp = PERS
            with ExitStack() as ph:
                k.es = ph
                G, SH = load_mod(l, 1, norm2_g)
                n = make_norm()
                rw = k.sb("rw", [128, 8, NE], F32)
                k.dma("sp", lambda e: e.dma_start(out=rw[:], in_=router_w.ap()[l].rearrange("(kc p) e -> p kc e", p=128)), writes=[rw])
                rb = k.sb("rb", [1, NE], F32)
                k.dma("sp", lambda e: e.dma_start(out=rb[:], in_=router_b.ap()[l:l + 1, :]), writes=[rb])
                h2b = k.sb("h2b", [128, NT, D], BF16)
                hT32 = [k.sb("hT32", [128, 8, 128], F32) for _ in range(2)]
                pl = [k.ps("pl", [128, NE]) for _ in range(2)]
                lg = [k.sb("lg", [128, NE], F32) for _ in range(2)]
                m8 = [k.sb("m8", [128, 8], F32) for _ in range(2)]
                ex = [k.sb("ex", [128, NE], F32) for _ in range(2)]
                sm = [k.sb("sm", [128, 4], F32) for _ in range(2)]
                Mall = k.sb("Mall", [128, NT, NE], F32)
                GWall = k.sb("GWall", [128, NT, NE], F32)
                idx8 = k.sb("idx8", [128, NT, 8], U32)
                def rt_a(t):
                    xt, h = norm_tile(n, xsrc, [xsrc.r(t)], t, G, SH)
                    k.op("act", lambda e: e.activation(h2b[:, t, :], h[:], AF.Identity), reads=[h], writes=[h2b.r(t)])
                    hT = hT32[t % 2]
                    transpose_to(n, h, [(lambda half: hT[:, half * 4:(half + 1) * 4, :], [hT.r(0)], "dve")])
                    p = pl[t % 2]
                    for kc in range(8):
                        k.op("pe", lambda e, kc=kc: e.matmul(p[:], lhsT=hT[:, kc, :], rhs=rw[:, kc, :], start=(kc == 0), stop=False),
                             reads=[hT.r(0), rw], writes=[p])
                    k.op("pe", lambda e: e.matmul(p[:], lhsT=ones_f[0:1, :], rhs=rb[0:1, :], start=False, stop=True), reads=[rb, ones_f], writes=[p])

                def rt_b(t):
                    p = pl[t % 2]
                    L, M8, E, SM = lg[t % 2], m8[t % 2], ex[t % 2], sm[t % 2]
                    k.op("dve", lambda e: e.tensor_copy(L[:], p[:]), reads=[p], writes=[L])
                    k.op("dve", lambda e: e.max(M8[:], L[:]), reads=[L], writes=[M8])
                    k.op("dve", lambda e: e.max_index(idx8[:, t, :], M8[:], L[:]), reads=[L, M8], writes=[idx8.r(t)])
                    k.op("dve", lambda e: e.tensor_scalar(Mall[:, t, :], L[:], M8[:, 3:4], None, ALU.is_ge), reads=[L, M8], writes=[Mall.r(t)])
                    k.op("dve", lambda e: e.tensor_scalar(SM[:, 0:1], M8[:, 0:1], -1.0, None, ALU.mult), reads=[M8], writes=[SM])
                    k.op("act", lambda e: e.activation(E[:], L[:], AF.Exp, bias=SM[:, 0:1], scale=1.0), reads=[L, SM], writes=[E])
                    k.op("dve", lambda e: e.tensor_tensor(E[:], E[:], Mall[:, t, :], ALU.mult), reads=[E, Mall.r(t)], writes=[E])
                    k.op("dve", lambda e: e.tensor_reduce(SM[:, 1:2], E[:], AX.X, ALU.add), reads=[E], writes=[SM])
                    k.op("dve", lambda e: e.reciprocal(SM[:, 2:3], SM[:, 1:2]), reads=[SM], writes=[SM])
                    k.op("dve", lambda e: e.tensor_scalar(GWall[:, t, :], E[:], SM[:, 2:3], None, ALU.mult), reads=[E, SM], writes=[GWall.r(t)])
                for t in range(NT + 1):
                    if t < NT:
                        rt_a(t)
                    if t >= 1:
                        rt_b(t - 1)
                Mres = [Mall.r(t) for t in range(NT)]
                pp = [k.ps("pp", [128, 512]) for _ in range(2)]
                ptot = [k.ps("ptot", [128, 512]) for _ in range(2)]
                Mflat = Mall[:].rearrange("p t e -> p (t e)")
                pre = k.sb("pre", [128, NT, NE], F32)
                tot = k.sb("tot", [128, NT, NE], F32)
                for hf in range(2):
                    k.op("pe", lambda e, hf=hf: e.matmul(pp[hf][:], lhsT=Uex, rhs=Mflat[:, hf * 512:(hf + 1) * 512], start=True, stop=True), reads=Mres + [cf], writes=[pp[hf]])
                    k.op("pe", lambda e, hf=hf: e.matmul(ptot[hf][:], lhsT=ones_f[:], rhs=Mflat[:, hf * 512:(hf + 1) * 512], start=True, stop=True), reads=Mres + [ones_f], writes=[ptot[hf]])
                    k.op("dve", lambda e, hf=hf: e.tensor_copy(pre[:].rearrange("p t e -> p (t e)")[:, hf * 512:(hf + 1) * 512], pp[hf][:]), reads=[pp[hf]], writes=[pre.r(hf)])
                    k.op("dve", lambda e, hf=hf: e.tensor_copy(tot[:].rearrange("p t e -> p (t e)")[:, hf * 512:(hf + 1) * 512], ptot[hf][:]), reads=[ptot[hf]], writes=[tot.r(hf)])
                off = k.sb("off", [128, NT + 1, NE], F32)
                k.op("dve", lambda e: e.memset(off[:, 0, :], 0.0), writes=[off])
                for t in range(NT):
                    k.op("dve", lambda e, t=t: e.tensor_tensor(off[:, t + 1, :], off[:, t, :], tot[:, t, :], ALU.add), reads=[off, tot.r(0), tot.r(1)], writes=[off])
                cnt = off[:, NT, :]
                cmp8 = k.sb("cmp8", [128, NE, 8], F32)
                nb = k.sb("nb", [128, 3, NE], F32)
                k.op("dve", lambda e: e.tensor_tensor(cmp8[:], cnt.unsqueeze(2).to_broadcast([128, NE, 8]), thr8.unsqueeze(1).to_broadcast([128, NE, 8]), ALU.is_gt),
                     reads=[off, cf], writes=[cmp8])
                k.op("dve", lambda e: e.tensor_reduce(nb[:, 0, :], cmp8[:], AX.X, ALU.add), reads=[cmp8], writes=[nb])
                k.op("dve", lambda e: e.tensor_tensor_scan(nb[:, 1, :], ones_f[:, 0:NE], nb[:, 0, :], 0.0, ALU.mult, ALU.add), reads=[nb, ones_f], writes=[nb])
                k.op("dve", lambda e: e.tensor_tensor(nb[:, 2, :], nb[:, 1, :], nb[:, 0, :], ALU.subtract), reads=[nb], writes=[nb])
                k.op("dve", lambda e: e.tensor_scalar(nb[:, 2, :], nb[:, 2, :], float(SB), None, ALU.mult), reads=[nb], writes=[nb])
                cmpj = k.sb("cmpj", [128, NSB, NE], F32)
                ej = k.sb("ej", [128, NSB], F32)
                k.op("dve", lambda e: e.tensor_tensor(cmpj[:], nb[:, 1, :].unsqueeze(1).to_broadcast([128, NSB, NE]), jidx.unsqueeze(2).to_broadcast([128, NSB, NE]), ALU.is_le),
                     reads=[nb, cf], writes=[cmpj])
                k.op("dve", lambda e: e.tensor_reduce(ej[:], cmpj[:], AX.X, ALU.add), reads=[cmpj], writes=[ej])
                k.op("dve", lambda e: e.tensor_scalar(ej[:], ej[:], float(NE - 1), None, ALU.min), reads=[ej], writes=[ej])
                k.op("dve", lambda e: e.tensor_tensor(pre[:], pre[:], off[:, 0:NT, :], ALU.add), reads=[pre.r(0), pre.r(1), off], writes=[pre.r(0), pre.r(1)])
                k.op("dve", lambda e: e.tensor_tensor(pre[:], pre[:], nb[:, 2, :].unsqueeze(1).to_broadcast([128, NT, NE]), ALU.add), reads=[pre.r(0), pre.r(1), nb], writes=[pre.r(0), pre.r(1)])
                idxf = k.sb("idxf", [128, NT, 8], F32)
                k.op("dve", lambda e: e.tensor_copy(idxf[:], idx8[:]), reads=[idx8.r(t) for t in range(NT)], writes=[idxf])
                oh = k.sb("oh", [128, NT, 4, NE], F32)
                prod = k.sb("prod", [128, NT, 4, NE], F32)
                dk = k.sb("dk", [128, NT, 4], F32)
                gk = k.sb("gk", [128, NT, 4], F32)
                GWres = [GWall.r(t) for t in range(NT)]
                k.op("dve", lambda e: e.tensor_tensor(oh[:], iota_e.unsqueeze(1).unsqueeze(1).to_broadcast([128, NT, 4, NE]),
                                                      idxf[:, :, 0:4].unsqueeze(3).to_broadcast([128, NT, 4, NE]), ALU.is_equal), reads=[idxf, cf], writes=[oh])
                k.op("dve", lambda e: e.tensor_tensor(prod[:], oh[:], pre[:].unsqueeze(2).to_broadcast([128, NT, 4, NE]), ALU.mult), reads=[oh, pre.r(0), pre.r(1)], writes=[prod])
                k.op("dve", lambda e: e.tensor_reduce(dk[:].rearrange("p t k -> p (t k)"), prod[:].rearrange("p t k e -> p (t k) e"), AX.X, ALU.add), reads=[prod], writes=[dk])
                k.op("dve", lambda e: e.tensor_tensor(prod[:], oh[:], GWall[:].unsqueeze(2).to_broadcast([128, NT, 4, NE]), ALU.mult), reads=[oh] + GWres, writes=[prod])
                k.op("dve", lambda e: e.tensor_reduce(gk[:].rearrange("p t k -> p (t k)"), prod[:].rearrange("p t k e -> p (t k) e"), AX.X, ALU.add), reads=[prod], writes=[gk])
                dki = k.sb("dki", [128, NT, 4], I32)
                k.op("dve", lambda e: e.tensor_copy(dki[:], dk[:]), reads=[dk], writes=[dki])
                widxf = k.sb("widxf", [128, NSB, 8], F32)
                k.op("dve", lambda e: e.scalar_tensor_tensor(widxf[:], ej[:].unsqueeze(2).to_broadcast([128, NSB, 8]), 1024.0, pidx.unsqueeze(1).to_broadcast([128, NSB, 8]), ALU.mult, ALU.add),
                     reads=[ej, cf], writes=[widxf])
                widx, ejb, dkp, gkp = PERS
                k.op("dve", lambda e: e.tensor_scalar(widxf[:], widxf[:], float(l * NE * D), None, ALU.add), reads=[widxf], writes=[widxf])
                k.op("dve", lambda e: e.tensor_copy(widx[:], widxf[:]), reads=[widxf], writes=[widx])
                k.op("dve", lambda e: e.tensor_copy(ejb[:], ej[:]), reads=[ej], writes=[ejb])
                k.op("dve", lambda e: e.tensor_copy(dkp[:], dki[:]), reads=[dki], writes=[dkp])
                k.op("dve", lambda e: e.tensor_copy(gkp[:], gk[:]), reads=[gk], writes=[gkp])
                for t in range(NT):
                    for kk in range(4):
                        k.dma("pool", lambda e, t=t, kk=kk: e.indirect_dma_start(
                            out=Xs[:, :], out_offset=bass.IndirectOffsetOnAxis(ap=dkp[:, t, kk:kk + 1], axis=0),
                            in_=h2b[:, t, :], in_offset=None), reads=[h2b.r(t), dkp], writes=[Xs.r((t, kk))])
                k.barrier()

            with ExitStack() as ph:
                k.es = ph
                Wgu = [k.sb("Wgu", [128, 8, 2 * D], BF16) for _ in range(2)]
                Wd = [k.sb("Wd", [128, 8, D], BF16) for _ in range(2)]
                Bd = k.sb("Bd", [NE, D], BF16)
                Bg32 = k.sb("Bg32", [NE, 2 * D], F32)
                k.dma("sp", lambda e: e.dma_start(out=Bg32[:], in_=moe_b_gu.ap()[l]), writes=[Bg32])
                BguT = k.sb("BguT", [128, 16, NE], F32)
                c78 = k.sb("c78", [128, 2], F32)
                k.op("dve", lambda e: e.memset(c78[:, 0:1], 7.0), writes=[c78])
                k.op("dve", lambda e: e.memset(c78[:, 1:2], 8.0), reads=[c78], writes=[c78])
                ohf = [k.sb("ohf", [128, NE], F32) for _ in range(2)]
                bprod = [k.sb("bprod", [128, 16, NE], F32) for _ in range(2)]
                bcol = [k.sb("bcol", [128, 16], F32) for _ in range(2)]
                k.dma("pool", lambda e: e.dma_start(out=Bd[:], in_=moe_b_down.ap()[l]), writes=[Bd])
                Xt = [k.sb("Xt", [128, 4, D], BF16) for _ in range(2)]
                XT = [k.sb("XT", [128, 8, SB], BF16) for _ in range(2)]
                AT = [k.sb("AT", [128, 8, SB], BF16) for _ in range(2)]
                OHD = [k.sb("OHD", [NE, 128], BF16) for _ in range(2)]
                gm = [k.sb("gm", [128, SB], F32) for _ in range(2)]
                gs = [k.sb("gs", [128, SB], F32) for _ in range(2)]
                um = [k.sb("um", [128, SB], F32) for _ in range(2)]
                Yt = [k.sb("Yt", [128, D], F32) for _ in range(2)]
                ptr = [k.ps("ptr", [128, 4, 128], BF16) for _ in range(2)]
                pg = [k.ps("pg", [128, SB]) for _ in range(2)]
                pu = [k.ps("pu", [128, SB]) for _ in range(2)]
                pd = [k.ps("pd", [128, 512]) for _ in range(2)]
                for fc in range(16):
                    k.op("pe", lambda e, fc=fc: e.transpose(pd[0][:, fc * NE:(fc + 1) * NE], Bg32[:, fc * 128:(fc + 1) * 128], ident_f[0:NE, 0:NE]), reads=[Bg32, cf], writes=[pd[0]])
                k.op("dve", lambda e: e.tensor_copy(BguT[:].rearrange("p a b -> p (a b)"), pd[0][:]), reads=[pd[0]], writes=[BguT])
                cnt_ = {"ci": 0, "yi": 0}
                wgu_flat = moe_w_gu.ap().rearrange("l r n -> (l r) n")
                wd_flat = moe_w_down.ap().rearrange("l r n -> (l r) n")

                def stage_load(j):
                    wg, wd, xt, ohd = Wgu[j % 2], Wd[j % 2], Xt[j % 2], OHD[j % 2]
                    k.dma("sp", lambda e: e.dma_start(out=xt[:], in_=Xs[j * SB:(j + 1) * SB, :].rearrange("(a p) d -> p a d", p=128)), reads=[], writes=[xt])
                    for kc in range(8):
                        k.dma("pool", lambda e, kc=kc: e.indirect_dma_start(
                            out=wg[:, kc, :], out_offset=None, in_=wgu_flat,
                            in_offset=bass.IndirectOffsetOnAxis(ap=widx[:, j, kc:kc + 1], axis=0)), reads=[widx], writes=[wg.r(kc)])
                    for kc in range(8):
                        k.dma("pool", lambda e, kc=kc: e.indirect_dma_start(
                            out=wd[:, kc, :], out_offset=None, in_=wd_flat,
                            in_offset=bass.IndirectOffsetOnAxis(ap=widx[:, j, kc:kc + 1], axis=0)), reads=[widx], writes=[wd.r(kc)])
                    OF_, BP_, BC_ = ohf[j % 2], bprod[j % 2], bcol[j % 2]
                    k.op("dve", lambda e: e.tensor_scalar(OF_[:], iota_e, ejb[:, j:j + 1], None, ALU.is_equal), reads=[ejb, cf], writes=[OF_])
                    k.op("dve", lambda e: e.tensor_tensor(BP_[:], BguT[:], OF_[:].unsqueeze(1).to_broadcast([128, 16, NE]), ALU.mult), reads=[BguT, OF_], writes=[BP_])
                    k.op("dve", lambda e: e.tensor_reduce(BC_[:], BP_[:], AX.X, ALU.add), reads=[BP_], writes=[BC_])
                    k.op("dve", lambda e: e.tensor_scalar(BC_[:, 8:16], BC_[:, 8:16], 1.0, None, ALU.add), reads=[BC_], writes=[BC_])
                    k.op("dve", lambda e: e.tensor_scalar(ohd[:], ejb[0:NE, j:j + 1].to_broadcast([NE, 128]), iota_p[0:NE, :], ALPHA, ALU.is_equal, ALU.mult),
                         reads=[ejb, cf], writes=[ohd])

                def stage_T(j):
                    xt, xT = Xt[j % 2], XT[j % 2]
                    for kc in range(8):
                        pt = ptr[kc % 2]
                        for a in range(4):
                            k.op("pe", lambda e, pt=pt, a=a, kc=kc: e.transpose(pt[:, a, :], xt[:, a, kc * 128:(kc + 1) * 128], ident_b), reads=[xt, cb], writes=[pt])
                        k.op("act", lambda e, pt=pt, kc=kc: e.activation(xT[:, kc, :], pt[:].rearrange("p a b -> p (a b)"), AF.Identity), reads=[pt], writes=[xT.r(kc)])

                def stage_gu(j):
                    wg, xT, aT = Wgu[j % 2], XT[j % 2], AT[j % 2]
                    BC_ = bcol[j % 2]
                    for gc in range(8):
                        ci = cnt_["ci"]
                        cnt_["ci"] += 1
                        PG, PU = pg[ci % 2], pu[ci % 2]
                        GM, GS, UM = gm[ci % 2], gs[ci % 2], um[ci % 2]
                        for (P_, fc) in ((PG, gc), (PU, gc + 8)):
                            for kc in range(8):
                                k.op("pe", lambda e, P_=P_, fc=fc, kc=kc: e.matmul(P_[:], lhsT=wg[:, kc, fc * 128:(fc + 1) * 128], rhs=xT[:, kc, :],
                                                                           start=(kc == 0), stop=(kc == 7)), reads=[wg.r(kc), xT.r(kc)], writes=[P_])
                        k.op("dve", lambda e, GM=GM, PG=PG, gc=gc: e.tensor_scalar(GM[:], PG[:], BC_[:, gc:gc + 1], c78[:, 0:1], ALU.add, ALU.min), reads=[PG, BC_, c78], writes=[GM])
                        k.op("act", lambda e, GM=GM, GS=GS: e.activation(GS[:], GM[:], AF.Silu, scale=ALPHA), reads=[GM], writes=[GS])
                        k.op("dve", lambda e, UM=UM, PU=PU, gc=gc: e.tensor_scalar(UM[:], PU[:], BC_[:, 8 + gc:9 + gc], c78[:, 1:2], ALU.add, ALU.min), reads=[PU, BC_, c78], writes=[UM])
                        k.op("dve", lambda e, UM=UM, GS=GS, gc=gc: e.scalar_tensor_tensor(aT[:, gc, :], UM[:], -6.0, GS[:], ALU.max, ALU.mult),
                             reads=[UM, GS], writes=[aT.r(gc)])

                def stage_down(j):
                    wd, aT, ohd = Wd[j % 2], AT[j % 2], OHD[j % 2]
                    for a in range(4):
                        yi = cnt_["yi"]
                        cnt_["yi"] += 1
                        Y = Yt[yi % 2]
                        for nh in range(2):
                            PD = pd[nh]
                            for gc in range(8):
                                k.op("pe", lambda e, PD=PD, gc=gc, a=a, nh=nh: e.matmul(PD[:], lhsT=aT[:, gc, a * 128:(a + 1) * 128], rhs=wd[:, gc, nh * 512:(nh + 1) * 512],
                                                                                start=(gc == 0), stop=False), reads=[aT.r(gc), wd.r(gc)], writes=[PD])
                            k.op("pe", lambda e, PD=PD, nh=nh: e.matmul(PD[:], lhsT=ohd[:], rhs=Bd[:, nh * 512:(nh + 1) * 512], start=False, stop=True),
                                 reads=[ohd, Bd], writes=[PD])
                            k.op("act", lambda e, PD=PD, Y=Y, nh=nh: e.activation(Y[:, nh * 512:(nh + 1) * 512], PD[:], AF.Identity, scale=1.0 / ALPHA), reads=[PD], writes=[Y.r(nh)])
                        k.dma("sp", lambda e, Y=Y, a=a: e.dma_start(out=Ys[j * SB + a * 128: j * SB + (a + 1) * 128, :], in_=Y[:]), reads=[Y.r(0), Y.r(1)], writes=[Ys.r((j, a))])

                stage_load(0)
                stage_T(0)
                for j in range(NSB):
                    if j + 1 < NSB:
                        stage_load(j + 1)
                    stage_gu(j)
                    if j + 1 < NSB:
                        stage_T(j + 1)
                    stage_down(j)
                k.barrier()

            with ExitStack() as ph:
                k.es = ph
                GT = load_gate(l, 1)
                xr = [k.sb("xr", [128, D], F32) for _ in range(2)]
                yk = [k.sb("yk", [128, D], F32) for _ in range(8)]
                acc = [k.sb("acc", [128, D], F32) for _ in range(2)]
                toks = []
                for t in range(NT):
                    X_, A_ = xr[t % 2], acc[t % 2]
                    k.dma("sp", lambda e, X_=X_, t=t: e.dma_start(out=X_[:], in_=xsrc[t * 128:(t + 1) * 128, :]), reads=[xsrc.r(t)], writes=[X_])
                    for kk in range(4):
                        Yk = yk[(t % 2) * 4 + kk]
                        k.dma("pool", lambda e, Yk=Yk, t=t, kk=kk: e.indirect_dma_start(
                            out=Yk[:], out_offset=None, in_=Ys[:, :], in_offset=bass.IndirectOffsetOnAxis(ap=dkp[:, t, kk:kk + 1], axis=0)),
                            reads=[dkp], writes=[Yk])
                        if kk == 0:
                            k.op("act", lambda e, A_=A_, Yk=Yk, t=t, kk=kk: e.activation(A_[:], Yk[:], AF.Identity, scale=gkp[:, t, kk:kk + 1]), reads=[Yk, gkp], writes=[A_])
                        else:
                            k.op("dve", lambda e, A_=A_, Yk=Yk, t=t, kk=kk: e.scalar_tensor_tensor(A_[:], Yk[:], gkp[:, t, kk:kk + 1], A_[:], ALU.mult, ALU.add), reads=[Yk, gkp, A_], writes=[A_])
                    k.op("dve", lambda e, A_=A_: e.tensor_tensor(A_[:], A_[:], GT[:], ALU.mult), reads=[A_, GT], writes=[A_])
                    k.op("dve", lambda e, A_=A_, X_=X_: e.tensor_tensor(A_[:], A_[:], X_[:], ALU.add), reads=[A_, X_], writes=[A_])
                    toks.append(k.dma("sp", lambda e, A_=A_, t=t: e.dma_start(out=xdst[t * 128:(t + 1) * 128, :], in_=A_[:]), reads=[A_], writes=[xdst.r(t)]))
                k.barrier()
            return toks


        def build_hT(l, xsrc, hT):
            with ExitStack() as ph2:
                k.es = ph2
                G, SH = load_mod(l, 0, norm1_g)
                n = make_norm()
                for t in range(NT):
                    xt, h = norm_tile(n, xsrc, [xsrc.r(t)], t, G, SH)
                    transpose_to(n, h, [(lambda half, t=t: hT[:, half * 4:(half + 1) * 4, t * 128:(t + 1) * 128], [hT.r(t)], "act")])
                k.barrier()

        def out_proj(l, w_out, xsrc, xdst):
            with ExitStack() as ph2:
                k.es = ph2
                GT = load_gate(l, 0)
                oT = k.sb("oT", [128, 8, S], BF16)
                k.dma("sp", lambda e: e.dma_start(out=oT[:], in_=OT[:, :].rearrange("(c p) s -> p c s", p=128)), writes=[oT])
                wo = k.sb("wo", [128, 8, D], BF16)
                k.dma("pool", lambda e: e.dma_start(out=wo[:], in_=w_out.ap().rearrange("(c p) n -> p c n", p=128)), writes=[wo])
                xr = [k.sb("xr", [128, D], F32) for _ in range(2)]
                x1 = [k.sb("x1", [128, D], F32) for _ in range(2)]
                po = [k.ps("po", [128, 512]) for _ in range(4)]
                for t in range(NT):
                    X_, X1 = xr[t % 2], x1[t % 2]
                    k.dma("sp", lambda e, X_=X_, t=t: e.dma_start(out=X_[:], in_=xsrc[t * 128:(t + 1) * 128, :]), reads=[xsrc.r(t)], writes=[X_])
                    for nh in range(2):
                        P_ = po[(t % 2) * 2 + nh]
                        for c_ in range(8):
                            k.op("pe", lambda e, P_=P_, c_=c_, t=t, nh=nh: e.matmul(P_[:], lhsT=oT[:, c_, t * 128:(t + 1) * 128], rhs=wo[:, c_, nh * 512:(nh + 1) * 512],
                                                                            start=(c_ == 0), stop=(c_ == 7)), reads=[oT, wo], writes=[P_])
                        k.op("dve", lambda e, P_=P_, X1=X1, nh=nh: e.tensor_tensor(X1[:, nh * 512:(nh + 1) * 512], P_[:], GT[:, nh * 512:(nh + 1) * 512], ALU.mult),
                             reads=[P_, GT], writes=[X1.r(nh)])
                        k.op("dve", lambda e, X_=X_, X1=X1, nh=nh: e.tensor_tensor(X1[:, nh * 512:(nh + 1) * 512], X1[:, nh * 512:(nh + 1) * 512], X_[:, nh * 512:(nh + 1) * 512], ALU.add),
                             reads=[X1.r(nh), X_], writes=[X1.r(nh)])
                    k.dma("sp", lambda e, X1=X1, t=t: e.dma_start(out=xdst[t * 128:(t + 1) * 128, :], in_=X1[:]), reads=[X1.r(0), X1.r(1)], writes=[xdst.r(t)])
                    if dbg is not None and l == DBG_LAYER:
                        k.dma("sp", lambda e, X1=X1, t=t: e.dma_start(out=dbg[t * 128:(t + 1) * 128, :], in_=X1[:]), reads=[X1.r(0), X1.r(1)], writes=[dbg.r(t)])
                k.barrier()

        def attn_layer(l, xsrc, xdst):
            with ExitStack() as ph:
                k.es = ph
                hT = k.sb("hT", [128, 8, S], BF16)
                build_hT(l, xsrc, hT)
                k.es = ph
                hTres = [hT.r(t) for t in range(NT)]
                Ct = k.sb("Ct", [128, S], BF16)
                St = k.sb("St", [128, S], BF16)
                epsc = k.sb("epsc", [128, 1], F32)
                k.op("dve", lambda e: e.memset(epsc[:], EPS), writes=[epsc])
                gcol = k.sb("gcol", [128, 3, 2], F32)
                with nc.allow_non_contiguous_dma(reason="tiny gain loads"):
                    for s_, src in ((0, a_q_gain), (1, a_k_gain)):
                        for hf in range(2):
                            k.dma("sp", lambda e, s_=s_, src=src, hf=hf: e.dma_start(out=gcol[hf * 64:(hf + 1) * 64, :, s_], in_=src.ap().rearrange("g e -> e g")), writes=[gcol])
                with ExitStack() as ph2:
                    k.es = ph2
                    PI = float(np.pi)
                    for c4 in range(4):
                        sl = slice(c4 * 1024, (c4 + 1) * 1024)
                        pi_ = k.sb("pi_", [128, 1024], I32)
                        ang = k.sb("ang", [128, 1024], F32)
                        yy = k.sb("yy", [128, 1024], F32)
                        ni = k.sb("ni", [128, 1024], I32)
                        k.dma("sp", lambda e, pi_=pi_, sl=sl: e.dma_start(out=pi_[:], in_=pos_in.ap()[:, sl].partition_broadcast(128)), writes=[pi_])
                        k.op("dve", lambda e, ang=ang, pi_=pi_: e.tensor_copy(ang[:], pi_[:]), reads=[pi_], writes=[ang])
                        k.op("dve", lambda e, ang=ang: e.tensor_scalar(ang[:], ang[:], invf, None, ALU.mult), reads=[ang, cf], writes=[ang])
                        for tab, shift in ((St, 0.0), (Ct, PI / 2)):
                            if shift != 0.0:
                                k.op("dve", lambda e, ang=ang, shift=shift: e.tensor_scalar(ang[:], ang[:], shift, None, ALU.add), reads=[ang], writes=[ang])
                            k.op("dve", lambda e, ang=ang, yy=yy: e.tensor_scalar(yy[:], ang[:], 1.0 / (2 * PI), None, ALU.mult), reads=[ang], writes=[yy])
                            k.op("dve", lambda e, ni=ni, yy=yy: e.tensor_copy(ni[:], yy[:]), reads=[yy], writes=[ni])
                            k.op("dve", lambda e, ni=ni, yy=yy: e.tensor_copy(yy[:], ni[:]), reads=[ni], writes=[yy])
                            k.op("dve", lambda e, ang=ang, yy=yy: e.scalar_tensor_tensor(yy[:], yy[:], -2 * PI, ang[:], ALU.mult, ALU.add), reads=[yy, ang], writes=[yy])
                            k.op("dve", lambda e, yy=yy: e.tensor_scalar(yy[:], yy[:], PI, -PI, ALU.min, ALU.max), reads=[yy], writes=[yy])
                            k.op("act", lambda e, tab=tab, yy=yy, sl=sl: e.activation(tab[:, sl], yy[:], AF.Sin), reads=[yy], writes=[tab])
                    k.barrier()
                k.es = ph
                accA = k.sb("accA", [128, S], F32)
                accB = k.sb("accB", [128, S], F32)
                Wq = [k.sb("Wq", [128, 8, 3, 128], BF16) for _ in range(2)]
                QK = [k.sb("QT", [128, S], BF16), k.sb("KT", [128, S], BF16)]
                Vt2 = k.sb("Vt2", [128, NT, 2, 128], BF16)
                k.op("dve", lambda e: e.memset(Vt2[:, :, :, 64:128], 1.0), writes=[Vt2.r(t_) for t_ in range(NT)])
                VT = k.sb("VT", [128, S], BF16)
                qs = [k.sb("qs", [128, 512], BF16) for _ in range(2)]
                sq = [k.sb("sq", [128, 512], BF16) for _ in range(2)]
                lnv = [k.sb("lnv", [128, 512], F32) for _ in range(2)]
                rstd = [k.sb("rstd", [128, 512], F32) for _ in range(2)]
                ta = [k.sb("ta", [128, 512], F32) for _ in range(2)]
                tb = [k.sb("tb", [128, 512], F32) for _ in range(2)]
                PT = [k.sb("PT", [128, 2, 128], BF16) for _ in range(4)]
                ost = [k.sb("ost", [128, 512], BF16) for _ in range(2)]
                rdn = [k.sb("rdn", [128, 512], F32) for _ in range(2)]
                pq = [k.ps("pq", [128, 512]) for _ in range(2)]
                pss = k.ps("pss", [128, 512])
                prq = k.ps("prq", [128, 512])
                pxa = k.ps("pxa", [128, 512])
                pxb = k.ps("pxb", [128, 512])
                pvt2 = [k.ps("pvt", [128, 128], BF16) for _ in range(2)]
                a_w4 = a_w_in.ap().rearrange("(c p) (gs h e) -> p c gs (h e)", p=128, gs=9, h=16)
                wi = 0
                qi = 0
                pti = 0
                for hp in range(8):
                    for g, d in enumerate((1, 4, 16)):
                        nbk = S // (128 * d)
                        W = Wq[wi % 2]
                        wi += 1
                        for c_ in range(8):
                            k.dma("pool", lambda e, W=W, g=g, hp=hp, c_=c_: e.dma_start(out=W[:, c_, :, :], in_=a_w4[:, c_, 3 * g:3 * g + 3, hp * 128:(hp + 1) * 128]), writes=[W.r(c_)])
                        Wres = [W.r(c_) for c_ in range(8)]

                        def tokv(ap2, r, n0, cnt, d=d):
                            return ap2.rearrange("p (u d) -> p u d", d=d)[:, n0 * 128:n0 * 128 + cnt, r]
                        def qk_a(tc, s_, bufi):
                            sl = slice(tc * 512, (tc + 1) * 512)
                            P_, QS, SQ = pq[bufi % 2], qs[bufi % 2], sq[bufi % 2]
                            for c_ in range(8):
                                k.op("pe", lambda e, c_=c_: e.matmul(P_[:], lhsT=W[:, c_, s_, :], rhs=hT[:, c_, sl], start=(c_ == 0), stop=(c_ == 7)),
                                     reads=Wres + hTres[tc * 4:(tc + 1) * 4], writes=[P_])
                            k.op("act", lambda e: e.activation(QS[:], P_[:], AF.Identity, scale=gcol[:, g, s_:s_ + 1]), reads=[P_, gcol], writes=[QS])
                            k.op("act", lambda e: e.activation(SQ[:], P_[:], AF.Square), reads=[P_], writes=[SQ])

                        def qk_b(tc, s_, bufi):
                            sl = slice(tc * 512, (tc + 1) * 512)
                            QS, SQ, LN, RS, TA, TB = qs[bufi % 2], sq[bufi % 2], lnv[bufi % 2], rstd[bufi % 2], ta[bufi % 2], tb[bufi % 2]
                            k.op("pe", lambda e: e.matmul(pss[:], lhsT=blk, rhs=SQ[:], start=True, stop=True), reads=[SQ, cb], writes=[pss])
                            k.op("pe", lambda e: e.matmul(prq[:], lhsT=RT, rhs=QS[:], start=True, stop=True), reads=[QS, cb], writes=[prq])
                            k.op("act", lambda e: e.activation(LN[:], pss[:], AF.Ln, bias=epsc[:, 0:1], scale=1.0), reads=[pss, epsc], writes=[LN])
                            k.op("act", lambda e: e.activation(RS[:], LN[:], AF.Exp, scale=-0.5), reads=[LN], writes=[RS])
                            k.op("dve", lambda e: e.tensor_tensor(TA[:], QS[:], Ct[:, sl], ALU.mult), reads=[QS, Ct], writes=[TA])
                            k.op("dve", lambda e: e.tensor_tensor(TB[:], prq[:], St[:, sl], ALU.mult), reads=[prq, St], writes=[TB])
                            k.op("dve", lambda e: e.tensor_tensor(TA[:], TA[:], TB[:], ALU.add), reads=[TA, TB], writes=[TA])
                            k.op("dve", lambda e: e.tensor_tensor(QK[s_][:, sl], TA[:], RS[:], ALU.mult), reads=[TA, RS], writes=[QK[s_].r(tc)])
                        qk_items = [(tc, s_) for tc in range(8) for s_ in range(2)]
                        for ii in range(len(qk_items) + 1):
                            if ii < len(qk_items):
                                qk_a(qk_items[ii][0], qk_items[ii][1], qi + ii)
                            if ii >= 1:
                                qk_b(qk_items[ii - 1][0], qk_items[ii - 1][1], qi + ii - 1)
                        qi += len(qk_items)
                        QKres = [[QK[s_].r(tc) for tc in range(8)] for s_ in range(2)]
                        for tc in range(8):
                            sl = slice(tc * 512, (tc + 1) * 512)
                            P_ = pq[qi % 2]
                            qi += 1
                            for c_ in range(8):
                                k.op("pe", lambda e, P_=P_, c_=c_, sl=sl: e.matmul(P_[:], lhsT=W[:, c_, 2, :], rhs=hT[:, c_, sl], start=(c_ == 0), stop=(c_ == 7)),
                                     reads=Wres + hTres[tc * 4:(tc + 1) * 4], writes=[P_])
                            if tc % 2 == 0:
                                k.op("act", lambda e, P_=P_, sl=sl: e.activation(VT[:, sl], P_[:], AF.Identity), reads=[P_], writes=[VT.r(tc)])
                            else:
                                k.op("dve", lambda e, P_=P_, sl=sl: e.tensor_copy(VT[:, sl], P_[:]), reads=[P_], writes=[VT.r(tc)])
                        VTres = [VT.r(tc) for tc in range(8)]
                        for ti in range(NT):
                            PV_ = pvt2[ti % 2]
                            r, n_ = ti // nbk, ti % nbk
                            k.op("pe", lambda e, r=r, n_=n_, PV_=PV_: e.transpose(PV_[:], tokv(VT[:], r, n_, 128), ident_b), reads=VTres + [cb], writes=[PV_])
                            if ti % 2 == 0:
                                k.op("act", lambda e, ti=ti, PV_=PV_: e.activation(Vt2[:, ti, :, 0:64], PV_[:].rearrange("p (h e) -> p h e", h=2), AF.Identity), reads=[PV_], writes=[Vt2.r(ti)])
                            else:
                                k.op("dve", lambda e, ti=ti, PV_=PV_: e.tensor_copy(Vt2[:, ti, :, 0:64], PV_[:].rearrange("p (h e) -> p h e", h=2)), reads=[PV_], writes=[Vt2.r(ti)])
                        items = []
                        for r in range(d):
                            for n0 in range(0, nbk, 4):
                                nblk = min(4, nbk - n0)
                                for h_ in range(2):
                                    for bi in range(nblk):
                                        items.append((r, n0, nblk, h_, bi, h_ == 1 and bi == nblk - 1))

                        def att_a(it, bufi):
                            r, n0, nblk, h_, bi, last = it
                            hs = slice(h_ * 64, (h_ + 1) * 64)
                            n_ = n0 + bi
                            PSb = (pq[0], pq[1], pss, prq)[bufi % 4]
                            psv = PSb[:, 0:256].rearrange("p (a b) -> p a b", a=2)
                            P_T = PT[bufi % 4]
                            qv = tokv(QK[0][hs, :], r, n_, 128)
                            k.op("pe", lambda e: e.matmul(psv[:, 1, :], lhsT=tokv(QK[1][hs, :], r, n_, 128), rhs=qv, start=True, stop=True),
                                 reads=QKres[0] + QKres[1], writes=[PSb])
                            if n_ > 0:
                                k.op("pe", lambda e: e.matmul(psv[:, 0, :], lhsT=tokv(QK[1][hs, :], r, n_ - 1, 128), rhs=qv, start=True, stop=True),
                                     reads=QKres[0] + QKres[1], writes=[PSb])
                                k.op("act", lambda e: e.activation(P_T[:], psv, AF.Exp, scale=0.125), reads=[PSb], writes=[P_T])
                                k.op("pool", lambda e: e.tensor_tensor(P_T[:], P_T[:], mask2, ALU.mult), reads=[P_T, cb2], writes=[P_T])
                            else:
                                k.op("act", lambda e: e.activation(P_T[:, 1, :], psv[:, 1, :], AF.Exp, scale=0.125), reads=[PSb], writes=[P_T])
                                k.op("pool", lambda e: e.tensor_tensor(P_T[:, 1, :], P_T[:, 1, :], mask2[:, 1, :], ALU.mult), reads=[P_T, cb2], writes=[P_T])

                        def att_b(it, bufi):
                            r, n0, nblk, h_, bi, last = it
                            n_ = n0 + bi
                            ti = r * nbk + n_
                            P_T = PT[bufi % 4]
                            PX = pxa if h_ == 0 else pxb
                            cs = slice(bi * 128, (bi + 1) * 128)
                            k.op("pe", lambda e: e.matmul(PX[:, cs], lhsT=Vt2[:, ti, h_, :], rhs=P_T[:, 1, :], start=True, stop=(n_ == 0)),
                                 reads=[P_T, Vt2.r(ti)], writes=[PX])
                            if n_ > 0:
                                k.op("pe", lambda e: e.matmul(PX[:, cs], lhsT=Vt2[:, ti - 1, h_, :], rhs=P_T[:, 0, :], start=False, stop=True),
                                     reads=[P_T, Vt2.r(ti - 1)], writes=[PX])
                            if last:
                                cw = nblk * 128
                                for (PX_, ACC) in ((pxa, accA), (pxb, accB)):
                                    av = tokv(ACC[:], r, n0, cw)
                                    if g == 0:
                                        if PX_ is pxa:
                                            k.op("act", lambda e, av=av, PX_=PX_: e.activation(av, PX_[:, 0:cw], AF.Identity), reads=[PX_], writes=[ACC])
                                        else:
                                            k.op("dve", lambda e, av=av, PX_=PX_: e.tensor_copy(av, PX_[:, 0:cw]), reads=[PX_], writes=[ACC])
                                    else:
                                        k.op("dve", lambda e, av=av, PX_=PX_: e.tensor_tensor(av, PX_[:, 0:cw], av, ALU.add), reads=[PX_, ACC], writes=[ACC])
                        LA = 3
                        for ii in range(len(items) + LA):
                            if ii < len(items):
                                att_a(items[ii], pti + ii)
                            if ii >= LA:
                                att_b(items[ii - LA], pti + ii - LA)
                        pti += len(items)
                    for hx, ACC in enumerate((accA, accB)):
                        for c8 in range(8):
                            sl = slice(c8 * 512, (c8 + 1) * 512)
                            RD, OS = rdn[c8 % 2], ost[c8 % 2]
                            PD_ = pss if c8 % 2 == 0 else prq
                            k.op("pe", lambda e, PD_=PD_, ACC=ACC, sl=sl: e.matmul(PD_[0:64, :], lhsT=ident_f[:, 64:128], rhs=ACC[:, sl], start=True, stop=True), reads=[ACC, cf], writes=[PD_])
                            k.op("act", lambda e, RD=RD, PD_=PD_: e.activation(RD[0:64, :], PD_[0:64, :], AF.Ln), reads=[PD_], writes=[RD])
                            k.op("act", lambda e, RD=RD: e.activation(RD[0:64, :], RD[0:64, :], AF.Exp, scale=-1.0), reads=[RD], writes=[RD])
                            k.op("dve", lambda e, RD=RD, OS=OS, ACC=ACC, sl=sl: e.tensor_tensor(OS[0:64, :], ACC[0:64, sl], RD[0:64, :], ALU.mult), reads=[RD, ACC], writes=[OS])
                            k.dma("sp", lambda e, OS=OS, hp=hp, hx=hx, sl=sl: e.dma_start(out=OT[hp * 128 + hx * 64:hp * 128 + (hx + 1) * 64, sl], in_=OS[0:64, :]), reads=[OS], writes=[OT.r((hp, hx, c8))])
                k.barrier()
            k.es = root
            out_proj(l, a_w_out, xsrc, xdst)

        def gla_layer(l, xsrc, xdst):
            with ExitStack() as ph:
                k.es = ph
                hT = k.sb("hT", [128, 8, S], BF16)
                build_hT(l, xsrc, hT)
                k.es = ph
                hTres = [hT.r(t) for t in range(NT)]
                Wb = k.sb("Wb", [128, 8, 3088], BF16)
                bw3 = b_w_in.ap().rearrange("(c p) n -> p c n", p=128)
                for c_ in range(8):
                    for hf in range(2):
                        k.dma("pool", lambda e, c_=c_, hf=hf: e.dma_start(out=Wb[:, c_, hf * 1544:(hf + 1) * 1544], in_=bw3[:, c_, hf * 1544:(hf + 1) * 1544]), writes=[Wb.r((c_, hf))])
                Wbres = [Wb.r((c_, hf)) for c_ in range(8) for hf in range(2)]
                Wg = k.sb("Wg", [16, 512], BF16)
                k.dma("pool", lambda e: e.dma_start(out=Wg[:], in_=b_w_gate_up.ap()), writes=[Wg])
                negb = k.sb("negb", [128, 4], F32)
                with nc.allow_non_contiguous_dma(reason="tiny bias load"):
                    k.dma("sp", lambda e: e.dma_start(out=negb[:], in_=b_gate_bias.ap().rearrange("o (h p) -> p (o h)", p=128)), writes=[negb])
                k.op("dve", lambda e: e.tensor_scalar(negb[:], negb[:], -1.0, None, ALU.mult), reads=[negb], writes=[negb])
                onec = k.sb("onec", [128, 1], F32)
                k.op("dve", lambda e: e.memset(onec[:], 1.0), writes=[onec])
                epsc = k.sb("epsc", [128, 1], F32)
                k.op("dve", lambda e: e.memset(epsc[:], EPS), writes=[epsc])
                ogain = k.sb("ogain", [128, 256], F32)
                k.dma("sp", lambda e: e.dma_start(out=ogain[:], in_=b_out_gain.ap().partition_broadcast(128)), writes=[ogain])
                rmask = k.sb("rmask", [128, 512], F32)
                k.op("dve", lambda e: e.memset(rmask[:], 1.0), writes=[rmask])
                k.op("dve", lambda e: e.memset(rmask[:].rearrange("p (a b) -> p a b", b=64)[:, :, 0:1], 0.0), reads=[rmask], writes=[rmask])
                S32 = [k.sb("S32", [128, 256], F32) for _ in range(4)]
                Sb = [k.sb("Sb", [128, 256], BF16) for _ in range(4)]
                for h_ in range(4):
                    k.op("dve", lambda e, h_=h_: e.memset(S32[h_][:], 0.0), writes=[S32[h_]])
                    k.op("dve", lambda e, h_=h_: e.memset(Sb[h_][:], 0.0), writes=[Sb[h_]])
                aT = k.sb("aT", [16, 512], BF16)
                e1 = [k.sb("e1", [128, 512], F32) for _ in range(2)]
                cs = [k.sb("cs", [128, 512], F32) for _ in range(2)]
                Ep = [k.sb("Ep", [128, 512], F32) for _ in range(2)]
                En = [k.sb("En", [128, 512], F32) for _ in range(2)]
                decs = k.sb("decs", [128, 4, 8], F32)
                QD = k.sb("QD", [128, 4, 512], BF16)
                KN = k.sb("KN", [128, 4, 512], BF16)
                vb = [k.sb("vb", [128, D], BF16) for _ in range(2)]
                sr = [k.sb("sr", [128, D], F32) for _ in range(2)]
                knT = [k.sb("knT", [128, 128], BF16) for _ in range(2)]
                Pm = [k.sb("Pm", [128, 128], BF16) for _ in range(2)]
                st2 = [k.sb("st2", [128, 4, 2], F32) for _ in range(2)]
                junk = k.sb("gjunk", [128, 256], BF16)
                onrm = [k.sb("onrm", [128, 256], F32) for _ in range(2)]
                ofin = [k.sb("ofin", [128, D], BF16) for _ in range(2)]
                oTs = [k.sb("oTs", [128, 8, 128], BF16) for _ in range(2)]
                pA = [k.ps("pA", [128, 512]) for _ in range(2)]
                pCs = [k.ps("pC", [128, 256]) for _ in range(2)]
                pkvs = [k.ps("pkv", [128, 256]) for _ in range(2)]
                patt = k.ps("patt", [128, 128])
                pOT = k.ps("pOT", [128, 8, 128], BF16)
                ai = 0
                ci = 0
                OTv = OT[:, :].rearrange("(c p) s -> p c s", p=128)
                vr_done = set()
                aic = [0]

                def proj_vr(t):
                    vr_done.add(t)
                    tl = slice(t * 128, (t + 1) * 128)
                    VB, SR = vb[t % 2], sr[t % 2]
                    for nh in range(2):
                        PA = pA[aic[0] % 2]
                        aic[0] += 1
                        for c_ in range(8):
                            k.op("pe", lambda e, c_=c_: e.matmul(PA[:], lhsT=hT[:, c_, tl], rhs=Wb[:, c_, 1024 + nh * 512:1024 + (nh + 1) * 512], start=(c_ == 0), stop=(c_ == 7)), reads=Wbres + [hT.r(t)], writes=[PA])
                        k.op("dve", lambda e: e.tensor_copy(VB[:, nh * 512:(nh + 1) * 512], PA[:]), reads=[PA], writes=[VB.r(nh)])
                    for nh in range(2):
                        PA = pA[aic[0] % 2]
                        aic[0] += 1
                        for c_ in range(8):
                            k.op("pe", lambda e, c_=c_: e.matmul(PA[:], lhsT=hT[:, c_, tl], rhs=Wb[:, c_, 2048 + nh * 512:2048 + (nh + 1) * 512], start=(c_ == 0), stop=(c_ == 7)), reads=Wbres + [hT.r(t)], writes=[PA])
                        k.op("act", lambda e: e.activation(SR[:, nh * 512:(nh + 1) * 512], PA[:], AF.Silu), reads=[PA], writes=[SR.r(nh)])
                for mc in range(8):
                    sl = slice(mc * 512, (mc + 1) * 512)
                    hr = hTres[mc * 4:(mc + 1) * 4]
                    PA = pA[ai % 2]
                    ai += 1
                    for c_ in range(8):
                        k.op("pe", lambda e, PA=PA, c_=c_, sl=sl: e.matmul(PA[0:16, :], lhsT=Wb[:, c_, 3072:3088], rhs=hT[:, c_, sl], start=(c_ == 0), stop=(c_ == 7)), reads=Wbres + hr, writes=[PA])
                    k.op("act", lambda e, PA=PA: e.activation(aT[:], PA[0:16, :], AF.Identity), reads=[PA], writes=[aT])
                    for h_ in range(4):
                        E1, CS, EP, EN = e1[h_ % 2], cs[h_ % 2], Ep[h_ % 2], En[h_ % 2]
                        PA = pA[ai % 2]
                        ai += 1
                        k.op("pe", lambda e, PA=PA, h_=h_: e.matmul(PA[:], lhsT=Wg[0:16, h_ * 128:(h_ + 1) * 128], rhs=aT[0:16, :], start=True, stop=True), reads=[Wg, aT], writes=[PA])
                        k.op("act", lambda e, PA=PA, E1=E1, h_=h_: e.activation(E1[:], PA[:], AF.Exp, bias=negb[:, h_:h_ + 1], scale=-1.0), reads=[PA, negb], writes=[E1])
                        k.op("act", lambda e, E1=E1: e.activation(E1[:], E1[:], AF.Ln, bias=onec[:, 0:1], scale=1.0), reads=[E1, onec], writes=[E1])
                        k.op("dve", lambda e, E1=E1, CS=CS: e.tensor_tensor_scan(CS[:], rmask[:], E1[:], 0.0, ALU.mult, ALU.add), reads=[E1, rmask], writes=[CS])
                        k.op("act", lambda e, CS=CS, EP=EP: e.activation(EP[:], CS[:], AF.Exp, scale=-1.0 / 16), reads=[CS], writes=[EP])
                        k.op("act", lambda e, CS=CS, EN=EN: e.activation(EN[:], CS[:], AF.Exp, scale=1.0 / 16), reads=[CS], writes=[EN])
                        k.op("dve", lambda e, EP=EP, h_=h_: e.tensor_copy(decs[:, h_, :], EP[:].rearrange("p (a b) -> p a b", b=64)[:, :, 63]), reads=[EP], writes=[decs.r(h_)])
                        PA = pA[ai % 2]
                        ai += 1
                        for c_ in range(8):
                            k.op("pe", lambda e, PA=PA, c_=c_, sl=sl, h_=h_: e.matmul(PA[:], lhsT=Wb[:, c_, h_ * 128:(h_ + 1) * 128], rhs=hT[:, c_, sl], start=(c_ == 0), stop=(c_ == 7)), reads=Wbres + hr, writes=[PA])
                        k.op("dve", lambda e, PA=PA, EP=EP, h_=h_: e.scalar_tensor_tensor(QD[:, h_, :], PA[:], float(128 ** -0.5), EP[:], ALU.mult, ALU.mult), reads=[PA, EP], writes=[QD.r(h_)])
                        PA = pA[ai % 2]
                        ai += 1
                        for c_ in range(8):
                            k.op("pe", lambda e, PA=PA, c_=c_, sl=sl, h_=h_: e.matmul(PA[:], lhsT=Wb[:, c_, 512 + h_ * 128:512 + (h_ + 1) * 128], rhs=hT[:, c_, sl], start=(c_ == 0), stop=(c_ == 7)), reads=Wbres + hr, writes=[PA])
                        k.op("dve", lambda e, PA=PA, EN=EN, h_=h_: e.tensor_tensor(KN[:, h_, :], PA[:], EN[:], ALU.mult), reads=[PA, EN], writes=[KN.r(h_)])
                    for t4 in range(4):
                        t = mc * 4 + t4
                        tl = slice(t * 128, (t + 1) * 128)
                        ml = slice(t4 * 128, (t4 + 1) * 128)
                        VB, SR, OF, OTS = vb[t % 2], sr[t % 2], ofin[t % 2], oTs[t % 2]
                        if t not in vr_done:
                            proj_vr(t)
                        for pair in range(2):
                            hh = (2 * pair, 2 * pair + 1)
                            for h_ in hh:
                                hb = h_ % 2
                                KT_, PM = knT[hb], Pm[hb]
                                vh = slice(h_ * 256, (h_ + 1) * 256)
                                vres = VB.r(h_ // 2)
                                k.op("pe", lambda e, h_=h_: e.transpose(pOT[:, 0, :], KN[:, h_, ml], ident_b), reads=[KN.r(h_), cb], writes=[pOT])
                                k.op("act", lambda e, KT_=KT_: e.activation(KT_[:], pOT[:, 0, :], AF.Identity), reads=[pOT], writes=[KT_])
                                k.op("pe", lambda e, h_=h_: e.matmul(patt[:], lhsT=KN[:, h_, ml], rhs=QD[:, h_, ml], start=True, stop=True), reads=[KN.r(h_), QD.r(h_)], writes=[patt])
                                k.op("dve", lambda e, PM=PM: e.tensor_tensor(PM[:], patt[:], gmask, ALU.mult), reads=[patt, cb2], writes=[PM])
                                k.op("pe", lambda e, PM=PM, vh=vh, hb=hb: e.matmul(pCs[hb][:], lhsT=PM[:], rhs=VB[:, vh], start=True, stop=False), reads=[PM, vres], writes=[pCs[hb]])
                            if pair == 0 and t + 1 < NT:
                                proj_vr(t + 1)
                            for half in range(2):
                                ps_ = slice(half * 64, (half + 1) * 64)
                                mq = slice(t4 * 128 + half * 64, t4 * 128 + (half + 1) * 64)
                                cidx = t4 * 2 + half
                                for h_ in hh:
                                    hb = h_ % 2
                                    KT_ = knT[hb]
                                    vh = slice(h_ * 256, (h_ + 1) * 256)
                                    vres = VB.r(h_ // 2)
                                    k.op("pe", lambda e, h_=h_, hb=hb: e.matmul(pCs[hb][ps_, :], lhsT=QD[:, h_, mq], rhs=Sb[h_][:], start=False, stop=(half == 1)),
                                         reads=[QD.r(h_), Sb[h_]], writes=[pCs[hb]])
                                    k.op("pe", lambda e, KT_=KT_, vh=vh, hb=hb: e.matmul(pkvs[hb][:], lhsT=KT_[ps_, :], rhs=VB[ps_, vh], start=True, stop=True), reads=[KT_, vres], writes=[pkvs[hb]])
                                for h_ in hh:
                                    hb = h_ % 2
                                    k.op("dve", lambda e, h_=h_, hb=hb: e.tensor_tensor(S32[h_][:], pkvs[hb][:], S32[h_][:], ALU.add), reads=[pkvs[hb], S32[h_]], writes=[S32[h_]])
                                for h_ in hh:
                                    k.op("act", lambda e, h_=h_: e.activation(Sb[h_][:], S32[h_][:], AF.Identity, scale=decs[:, h_, cidx:cidx + 1]), reads=[S32[h_], decs.r(h_)], writes=[Sb[h_]])
                                for h_ in hh:
                                    k.op("dve", lambda e, h_=h_: e.tensor_scalar(S32[h_][:], S32[h_][:], decs[:, h_, cidx:cidx + 1], None, ALU.mult), reads=[S32[h_], decs.r(h_)], writes=[S32[h_]])
                            ST2 = st2[pair]
                            for h_ in hh:
                                hb = h_ % 2
                                k.op("act", lambda e, hb=hb: e.activation(junk[:], pCs[hb][:], AF.Square, accum_out=ST2[:, 0, hb:hb + 1]), reads=[pCs[hb]], writes=[junk, ST2])
                            k.op("dve", lambda e: e.tensor_scalar(ST2[:, 1, :], ST2[:, 0, :], 1.0 / 256, EPS, ALU.mult, ALU.add), reads=[ST2], writes=[ST2])
                            k.op("act", lambda e: e.activation(ST2[:, 2, :], ST2[:, 1, :], AF.Ln), reads=[ST2], writes=[ST2])
                            k.op("act", lambda e: e.activation(ST2[:, 3, :], ST2[:, 2, :], AF.Exp, scale=-0.5), reads=[ST2], writes=[ST2])
                            for h_ in hh:
                                hb = h_ % 2
                                ON = onrm[hb]
                                vh = slice(h_ * 256, (h_ + 1) * 256)
                                k.op("dve", lambda e, hb=hb, ON=ON: e.scalar_tensor_tensor(ON[:], pCs[hb][:], ST2[:, 3, hb:hb + 1], ogain[:], ALU.mult, ALU.mult), reads=[pCs[hb], ST2, ogain], writes=[ON])
                                k.op("dve", lambda e, ON=ON, vh=vh, h_=h_: e.tensor_tensor(OF[:, vh], ON[:], SR[:, vh], ALU.mult), reads=[ON, SR.r(h_ // 2)], writes=[OF.r(h_)])
                        for c_ in range(8):
                            k.op("pe", lambda e, c_=c_, OF=OF: e.transpose(pOT[:, c_, :], OF[:, c_ * 128:(c_ + 1) * 128], ident_b), reads=[OF.r(c_ // 2), cb], writes=[pOT])
                        k.op("act", lambda e, OTS=OTS: e.activation(OTS[:], pOT[:], AF.Identity), reads=[pOT], writes=[OTS])
                        k.dma("sp", lambda e, OTS=OTS, tl=tl: e.dma_start(out=OTv[:, :, tl], in_=OTS[:]), reads=[OTS], writes=[OT.r(("g", t))])
                k.barrier()
            k.es = root
            out_proj(l, b_w_out, xsrc, xdst)

        XIN = Buf(x_in, "x")
        attn_layer(0, XIN, xs1)
        k.es = root
        moe_layer(0, xs1, xs2)
        k.es = root
        gla_layer(1, xs2, xs1)
        k.es = root
        moe_layer(1, xs1, outb)
        k.es = root
        k.barrier()
        print("instructions:", k.ninst)
    return nc


def make_consts():
    c = np.zeros((128, 1024), np.float32)
    p = np.arange(128)
    c[:, 0:128] = np.eye(128)
    c[:, 128:256] = (p[:, None] < p[None, :]).astype(np.float32)
    c[:, 256:288] = np.arange(32)[None, :]
    c[:, 288:296] = (np.arange(8) * SB)[None, :]
    c[:, 296:360] = np.arange(64)[None, :]
    c[:, 360:368] = np.arange(8)[None, :] * 128 + p[:, None]
    c[:, 368] = p
    half = 32
    inv = (10000.0 ** (-np.arange(half, dtype=np.float32) / half)).astype(np.float32)
    c[:, 369] = inv[p % 32]
    RT = np.zeros((128, 128), np.float32)
    for hh in range(2):
        for e in range(64):
            if e < 32:
                RT[hh * 64 + e + 32, hh * 64 + e] = -1.0
            else:
                RT[hh * 64 + e - 32, hh * 64 + e] = 1.0
    c[:, 384:512] = RT
    blk = np.zeros((128, 128), np.float32)
    blk[:64, :64] = 1.0 / 64
    blk[64:, 64:] = 1.0 / 64
    c[:, 512:640] = blk
    c[:, 640:768] = np.where(p[:, None] <= p[None, :], 0.0, NEGM)
    c[:, 768:896] = np.where(p[:, None] >= p[None, :], 0.0, NEGM)
    c[:, 896:1024] = np.eye(128)
    return c


def make_consts2():
    p = np.arange(128)
    c = np.zeros((128, 384), np.float32)
    c[:, 0:128] = ((p[:, None] // 64 == p[None, :] // 64) & (p[:, None] <= p[None, :]))
    c[:, 128:256] = (p[:, None] >= p[None, :])
    c[:, 256:384] = (p[:, None] <= p[None, :])
    return c


_NC = None
_DEBUG = False


def kernel(**inp):
    global _NC
    if _NC is None:
        _NC = build_program(debug=_DEBUG)
    cst = make_consts()
    shared = {
        "ada_w": inp["ada_w"], "ada_b": inp["ada_b"], "norm1_g": inp["norm1_g"], "norm2_g": inp["norm2_g"],
        "a_w_in": inp["a_w_in"][0], "a_q_gain": inp["a_q_gain"][0], "a_k_gain": inp["a_k_gain"][0],
        "a_w_out": inp["a_w_out"][0], "b_w_in": inp["b_w_in"][0], "b_w_gate_up": inp["b_w_gate_up"][0],
        "b_gate_bias": inp["b_gate_bias"], "b_out_gain": inp["b_out_gain"], "b_w_out": inp["b_w_out"][0],
        "router_w": inp["router_w"], "router_b": inp["router_b"],
        "moe_w_gu": inp["moe_w_gu"].reshape(2, NE * D, 2 * D), "moe_b_gu": inp["moe_b_gu"],
        "moe_w_down": inp["moe_w_down"].reshape(2, NE * D, D), "moe_b_down": inp["moe_b_down"],
        "cst": cst, "cst2": make_consts2(),
    }
    shared = {kk: np.ascontiguousarray(v, dtype=np.float32) for kk, v in shared.items()}
    in_maps = []
    for b in range(8):
        m = dict(shared)
        m["x"] = np.ascontiguousarray(inp["x"][b])
        m["c"] = np.ascontiguousarray(inp["c"][b:b + 1])
        m["pos"] = np.ascontiguousarray(inp["positions"][b:b + 1]).astype(np.int32)
        in_maps.append(m)
    if _DEBUG:
        return run_bass_kernel_spmd(_NC, in_maps, core_ids=list(range(8)))
    res = run_bass_kernel_spmd(_NC, in_maps, core_ids=list(range(8)))
    return np.stack([res.results[b]["out"] for b in range(8)], axis=0)
```
